# Optimizing a Trainium2 kernel written in Bass

```python
import math
import jax, jax.numpy as jnp
from jax import lax
import numpy as np

D_MODEL = 2048
BATCH = 1
SEQ = 8192
DEPTH = 2

HEAD_DIM = 128
SB_HEADS = 8
FOX_HEADS = 8
SB_WIDTH = SB_HEADS * HEAD_DIM
FOX_WIDTH = FOX_HEADS * HEAD_DIM
N_META = 16
BLOCK_Q = 128
N_PAD = BLOCK_Q - N_META
PREFIX = N_PAD + N_META
N_EXPERTS = 16
N_GROUPS = 4
EXPERTS_PER_GROUP = N_EXPERTS // N_GROUPS
TOP_K = 2
EXPERT_FF = 1024
MOE_BLOCK = 256
ALPHA = (2 * DEPTH) ** 0.25
BETA = (8 * DEPTH) ** -0.25
LN_EPS = 1e-5
NEG = -1e30

_Q_SB = SB_WIDTH
_K_SB = 2 * SB_WIDTH
_V_SB = 3 * SB_WIDTH
_Q_FX = _V_SB + FOX_WIDTH
_K_FX = _V_SB + 2 * FOX_WIDTH
_V_FX = _V_SB + 3 * FOX_WIDTH
_F_FX = _V_FX + FOX_HEADS
_G_SB = _F_FX + D_MODEL
IN_COLS = _G_SB + D_MODEL
SPLITS = (_Q_SB, _K_SB, _V_SB, _Q_FX, _K_FX, _V_FX, _F_FX, _G_SB)

kernel_name = "stickbreak_fox_grouped_moe_hybrid"


def layer_norm(x, g, b):
    xf = x.astype(jnp.float32)
    mu = jnp.mean(xf, axis=-1, keepdims=True)
    var = jnp.mean(jnp.square(xf - mu), axis=-1, keepdims=True)
    y = (xf - mu) * lax.rsqrt(var + LN_EPS) * g.astype(jnp.float32) + b.astype(jnp.float32)
    return y.astype(x.dtype)


def split_heads(t, n_heads):
    b, l, _ = t.shape
    return t.reshape(b, l, n_heads, HEAD_DIM).transpose(0, 2, 1, 3)


def merge_heads(t):
    b, h, l, d = t.shape
    return t.transpose(0, 2, 1, 3).reshape(b, l, h * d)


def stick_breaking_attention(q, k, v, key_valid):
    b, h, l, dh = q.shape
    scale = dh ** -0.5
    kpos = jnp.arange(l)

    def block(i):
        qb = lax.dynamic_slice_in_dim(q, i * BLOCK_Q, BLOCK_Q, axis=2)
        qpos = i * BLOCK_Q + jnp.arange(BLOCK_Q)
        vis = (kpos[None, :] < qpos[:, None]) & key_valid[None, :]
        z = jnp.einsum('bhqd,bhkd->bhqk', qb, k, preferred_element_type=jnp.float32) * scale
        log_beta = jax.nn.log_sigmoid(z)
        log_keep = jnp.where(vis, jax.nn.log_sigmoid(-z), 0.0)
        suffix = lax.cumsum(log_keep, axis=3, reverse=True)
        after = jnp.concatenate([suffix[..., 1:], jnp.zeros_like(suffix[..., :1])], axis=3)
        w = jnp.where(vis, jnp.exp(log_beta + after), 0.0)
        return jnp.einsum('bhqk,bhkd->bhqd', w.astype(v.dtype), v)

    out = lax.map(block, jnp.arange(l // BLOCK_Q))
    return out.transpose(1, 2, 0, 3, 4).reshape(b, h, l, dh)


def forgetting_attention(q, k, v, log_f_cum, key_valid):
    b, h, l, dh = q.shape
    scale = dh ** -0.5
    kpos = jnp.arange(l)

    def block(i):
        qb = lax.dynamic_slice_in_dim(q, i * BLOCK_Q, BLOCK_Q, axis=2)
        cq = lax.dynamic_slice_in_dim(log_f_cum, i * BLOCK_Q, BLOCK_Q, axis=2)
        qpos = i * BLOCK_Q + jnp.arange(BLOCK_Q)
        vis = (kpos[None, :] <= qpos[:, None]) & key_valid[None, :]
        s = jnp.einsum('bhqd,bhkd->bhqk', qb, k, preferred_element_type=jnp.float32) * scale
        s = s + cq[..., :, None] - log_f_cum[..., None, :]
        p = jax.nn.softmax(jnp.where(vis, s, NEG), axis=-1)
        return jnp.einsum('bhqk,bhkd->bhqd', p.astype(v.dtype), v)

    out = lax.map(block, jnp.arange(l // BLOCK_Q))
    return out.transpose(1, 2, 0, 3, 4).reshape(b, h, l, dh)


def hybrid_mixer(h, w_in, b_forget, w_branch_sb, w_branch_fox, w_out, key_valid):
    proj = h @ w_in
    q_sb, k_sb, v_sb, q_fx, k_fx, v_fx, f_logit, g_sb, g_fx = jnp.split(proj, SPLITS, axis=-1)
    log_f = jax.nn.log_sigmoid((f_logit + b_forget).astype(jnp.float32))
    log_f = jnp.where(key_valid[None, :, None], log_f, 0.0)
    log_f_cum = jnp.cumsum(log_f, axis=1).transpose(0, 2, 1)
    y_sb = stick_breaking_attention(split_heads(q_sb, SB_HEADS), split_heads(k_sb, SB_HEADS),
                                    split_heads(v_sb, SB_HEADS), key_valid)
    y_fx = forgetting_attention(split_heads(q_fx, FOX_HEADS), split_heads(k_fx, FOX_HEADS),
                                split_heads(v_fx, FOX_HEADS), log_f_cum, key_valid)
    merged = (jax.nn.sigmoid(g_sb) * (merge_heads(y_sb) @ w_branch_sb)
              + jax.nn.sigmoid(g_fx) * (merge_heads(y_fx) @ w_branch_fox))
    return merged @ w_out


def grouped_top2_route(hf, w_router, router_bias):
    n = hf.shape[0]
    aff = jax.nn.sigmoid(jnp.dot(hf, w_router, preferred_element_type=jnp.float32))
    sel = aff + router_bias.astype(jnp.float32)
    grp_score = lax.top_k(sel.reshape(n, N_GROUPS, EXPERTS_PER_GROUP), TOP_K)[0].sum(-1)
    best_group = jnp.argmax(grp_score, axis=-1)
    in_group = (jnp.arange(N_EXPERTS) // EXPERTS_PER_GROUP)[None, :] == best_group[:, None]
    _, idx = lax.top_k(jnp.where(in_group, sel, -jnp.inf), TOP_K)
    w = jnp.take_along_axis(aff, idx, axis=-1)
    return idx, w / jnp.sum(w, axis=-1, keepdims=True)


def moe_ffn(h, w_router, router_bias, w_gate, w_up, w_down):
    b, l, d = h.shape
    n = b * l
    hf = h.reshape(n, d)
    idx, wts = grouped_top2_route(hf, w_router, router_bias)
    n_slots = n * TOP_K
    slot_e = idx.reshape(-1)
    slot_tok = jnp.repeat(jnp.arange(n, dtype=jnp.int32), TOP_K)
    slot_w = wts.reshape(-1)
    order = jnp.argsort(slot_e)
    e_sorted = slot_e[order]
    counts = jnp.bincount(slot_e, length=N_EXPERTS)
    padded = (counts + MOE_BLOCK - 1) // MOE_BLOCK * MOE_BLOCK
    start = jnp.cumsum(counts) - counts
    pend = jnp.cumsum(padded)
    pstart = pend - padded
    dest = pstart[e_sorted] + jnp.arange(n_slots) - start[e_sorted]
    n_blocks = -(-(n_slots + N_EXPERTS * (MOE_BLOCK - 1)) // MOE_BLOCK)
    p = n_blocks * MOE_BLOCK
    buf_tok = jnp.zeros((p,), jnp.int32).at[dest].set(slot_tok[order])
    buf_w = jnp.zeros((p,), h.dtype).at[dest].set(slot_w[order].astype(h.dtype))
    block_e = jnp.minimum(jnp.searchsorted(pend, jnp.arange(n_blocks) * MOE_BLOCK, side='right'),
                          N_EXPERTS - 1)
    xin = hf[buf_tok].reshape(n_blocks, MOE_BLOCK, d)

    def expert_block(args):
        xb, e = args
        return (jax.nn.silu(xb @ w_gate[e]) * (xb @ w_up[e])) @ w_down[e]

    yb = lax.map(expert_block, (xin, block_e)).reshape(p, d)
    y = jnp.zeros((n, d), h.dtype).at[buf_tok].add(yb * buf_w[:, None])
    return y.reshape(b, l, d)


def setup_inputs(seed: int = 0) -> dict:
    key = jax.random.key(seed)
    ks = jax.random.split(key, 20)
    f32 = jnp.float32
    d = D_MODEL
    col_scale = jnp.concatenate([
        jnp.ones((2 * SB_WIDTH,), f32), jnp.full((SB_WIDTH,), BETA, f32),
        jnp.ones((2 * FOX_WIDTH,), f32), jnp.full((FOX_WIDTH,), BETA, f32),
        jnp.ones((FOX_HEADS + 2 * d,), f32)])
    return {
        "x": jax.random.normal(ks[0], (BATCH, SEQ, d), f32),
        "meta_tokens": jax.random.normal(ks[1], (N_META, d), f32),
        "ln_in_g": 1.0 + 0.02 * jax.random.normal(ks[2], (d,), f32),
        "ln_in_b": 0.02 * jax.random.normal(ks[3], (d,), f32),
        "w_in": jax.random.normal(ks[4], (DEPTH, d, IN_COLS), f32) * (d ** -0.5) * col_scale,
        "b_forget": jax.random.uniform(ks[5], (DEPTH, FOX_HEADS), f32, 1.0, 4.0),
        "w_branch_sb": jax.random.normal(ks[6], (DEPTH, SB_WIDTH, d), f32) * SB_WIDTH ** -0.5,
        "w_branch_fox": jax.random.normal(ks[7], (DEPTH, FOX_WIDTH, d), f32) * FOX_WIDTH ** -0.5,
        "w_out": jax.random.normal(ks[8], (DEPTH, d, d), f32) * (d ** -0.5) * BETA,
        "ln_mix_g": 1.0 + 0.02 * jax.random.normal(ks[9], (DEPTH, d), f32),
        "ln_mix_b": 0.02 * jax.random.normal(ks[10], (DEPTH, d), f32),
        "w_router": jax.random.normal(ks[11], (d, N_EXPERTS), f32) * d ** -0.5,
        "router_bias": 0.01 * jax.random.normal(ks[12], (N_EXPERTS,), f32),
        "w_gate": jax.random.normal(ks[13], (DEPTH, N_EXPERTS, d, EXPERT_FF), f32) * d ** -0.5,
        "w_up": jax.random.normal(ks[14], (DEPTH, N_EXPERTS, d, EXPERT_FF), f32) * d ** -0.5,
        "w_down": jax.random.normal(ks[15], (DEPTH, N_EXPERTS, EXPERT_FF, d), f32) * (EXPERT_FF ** -0.5) * BETA,
        "ln_ffn_g": 1.0 + 0.02 * jax.random.normal(ks[16], (DEPTH, d), f32),
        "ln_ffn_b": 0.02 * jax.random.normal(ks[17], (DEPTH, d), f32),
    }


def reference(x, meta_tokens, ln_in_g, ln_in_b, w_in, b_forget, w_branch_sb, w_branch_fox, w_out,
              ln_mix_g, ln_mix_b, w_router, router_bias, w_gate, w_up, w_down, ln_ffn_g, ln_ffn_b):
    b, s, d = x.shape
    l = s + PREFIX
    pad = jnp.zeros((b, N_PAD, d), x.dtype)
    meta = jnp.broadcast_to(meta_tokens.astype(x.dtype)[None], (b, N_META, d))
    h = jnp.concatenate([pad, meta, x], axis=1)
    key_valid = jnp.arange(l) >= N_PAD
    h = layer_norm(h, ln_in_g, ln_in_b)
    for i in range(DEPTH):
        mix = hybrid_mixer(h, w_in[i], b_forget[i], w_branch_sb[i], w_branch_fox[i], w_out[i], key_valid)
        h = layer_norm(ALPHA * h + mix, ln_mix_g[i], ln_mix_b[i])
        ffn = moe_ffn(h, w_router, router_bias, w_gate[i], w_up[i], w_down[i])
        h = layer_norm(ALPHA * h + ffn, ln_ffn_g[i], ln_ffn_b[i])
    return h[:, PREFIX:, :]
```

```python
import numpy as np
import ml_dtypes
import concourse.bass as bass
import concourse.mybir as mybir
from concourse.bass_utils import run_bass_kernel_spmd

F32 = mybir.dt.float32
BF16 = mybir.dt.bfloat16
I32 = mybir.dt.int32
AF = mybir.ActivationFunctionType
ALU = mybir.AluOpType
AX = mybir.AxisListType

D = 2048
SEQ = 8192
L = SEQ + 128
NBLK = L // 128
DH = 128
NPAD = 112
NE = 16
FF = 1024
CAP = 256
ALPHA = 4.0 ** 0.25
EPS = 1e-5
NCORE = 8
TB = 9
NT = TB * 128


class Prog:
    SELFSYNC = ("act", "dve", "pool")

    def __init__(self, nc, n_dma_sems=12):
        self.nc = nc
        self.ops = []
        self.res_w = {}
        self.res_r = {}
        self.n_dma_sems = n_dma_sems

    def add(self, eng, fn, reads=(), writes=(), dma=False):
        idx = len(self.ops)
        deps = set()
        ex = [r for r in reads if isinstance(r, str) and r.startswith("bank")]
        if ex:
            reads = [r for r in reads if r not in ex]
            writes = list(writes) + [r for r in ex if r not in writes]
        for r in reads:
            w = self.res_w.get(r)
            if w is not None:
                deps.add(w)
        for w_ in writes:
            w = self.res_w.get(w_)
            if w is not None:
                deps.add(w)
            for rd in self.res_r.get(w_, ()):
                deps.add(rd)
        deps.discard(idx)
        self.ops.append(dict(eng=eng, fn=fn, deps=deps, dma=dma, sig=dma))
        for r in reads:
            self.res_r.setdefault(r, []).append(idx)
        for w_ in writes:
            self.res_w[w_] = idx
            self.res_r[w_] = []
        return idx

    def pe(self, fn, reads=(), writes=()):
        return self.add("pe", fn, reads, writes)

    def act(self, fn, reads=(), writes=()):
        return self.add("act", fn, reads, writes)

    def dve(self, fn, reads=(), writes=()):
        return self.add("dve", fn, reads, writes)

    def pool(self, fn, reads=(), writes=()):
        return self.add("pool", fn, reads, writes)

    def dma(self, fn, reads=(), writes=(), q="sp"):
        return self.add(q, fn, reads, writes, dma=True)

    def barrier(self):
        last = {}
        dmas = set()
        for i, x in enumerate(self.ops):
            if x["fn"] is None:
                continue
            if x["dma"]:
                dmas.add(i)
            else:
                last[x["eng"]] = i
        deps = set(last.values()) | dmas
        for e in ("pe", "act", "dve", "pool", "sp"):
            self.ops.append(dict(eng=e, fn=None, deps=set(deps), dma=False, sig=False, bar=True))

    def emit(self, final_waits=True):
        nc = self.nc
        ops = self.ops
        for x in ops:
            for d in x["deps"]:
                dd = ops[d]
                if dd["dma"]:
                    continue
                if dd["eng"] != x["eng"] or x["dma"] or dd["eng"] in self.SELFSYNC or x.get("bar"):
                    dd["sig"] = True
        engs = ["pe", "act", "dve", "pool", "sp"]
        import contextlib
        with contextlib.ExitStack() as st:
            esem = {e: st.enter_context(nc.semaphore("sem_" + e)) for e in engs}
            dsem = {q: [st.enter_context(nc.semaphore("dsem_%s_%d" % (q, j)))
                        for j in range(self.n_dma_sems)] for q in ("sp", "pool", "act")}
            ecount = {e: 0 for e in engs}
            dnext = {q: 0 for q in dsem}
            duse = {q: [0] * self.n_dma_sems for q in dsem}
            for x in ops:
                if x["dma"]:
                    q = x["eng"]
                    j = dnext[q]
                    dnext[q] = (j + 1) % self.n_dma_sems
                    duse[q][j] += 1
                    x["sem"] = dsem[q][j]
                    x["val"] = 16 * duse[q][j]
                    x["prev"] = 16 * (duse[q][j] - 1)
                elif x["sig"]:
                    ecount[x["eng"]] += 1
                    x["sem"] = esem[x["eng"]]
                    x["val"] = ecount[x["eng"]]
            block = st.enter_context(nc.Block())
            per_eng = {e: [x for x in ops if x["eng"] == e] for e in engs}
            last_dma = [x for x in ops if x["dma"]]

            def run(e, eng):
                waited = {}

                def wait(sem, val):
                    key = id(sem)
                    if waited.get(key, 0) >= val:
                        return
                    waited[key] = val
                    eng.wait_ge(sem, val)

                for x in per_eng[e]:
                    for d in sorted(x["deps"]):
                        dd = ops[d]
                        if (not dd["dma"]) and dd["eng"] == e and not x["dma"] and e not in self.SELFSYNC \
                                and not x.get("bar"):
                            continue
                        wait(dd["sem"], dd["val"])
                    if x["fn"] is None:
                        continue
                    if x["dma"] and x["prev"] > 0:
                        wait(x["sem"], x["prev"])
                    ins = x["fn"](eng)
                    if x["dma"]:
                        ins.then_inc(x["sem"], 16)
                    elif x["sig"]:
                        ins.then_inc(x["sem"], 1)
                if e == "sp" and final_waits:
                    for q in dsem:
                        for j in range(self.n_dma_sems):
                            if duse[q][j] > 0:
                                eng.wait_ge(dsem[q][j], 16 * duse[q][j])

            @block.tensor
            def _(eng):
                run("pe", eng)

            @block.scalar
            def _(eng):
                run("act", eng)

            @block.vector
            def _(eng):
                run("dve", eng)

            @block.gpsimd
            def _(eng):
                run("pool", eng)

            @block.sync
            def _(eng):
                run("sp", eng)


class Ctx:
    def __init__(self, nc, stack):
        self.nc = nc
        self.stack = stack
        self.n = 0

    def sb(self, shape, dt, name=None):
        self.n += 1
        return self.stack.enter_context(self.nc.sbuf_tensor(name or ("t%d" % self.n), list(shape), dt))

    def ps(self, shape, dt=F32, name=None):
        self.n += 1
        return self.stack.enter_context(self.nc.psum_tensor(name or ("p%d" % self.n), list(shape), dt))


def make_consts(P, cx, names):
    c = {}

    def tri(name, cmp_op, dt, val, flip=False, n=128):
        sgn = -1 if flip else 1
        tf = cx.sb([128, n], F32, name + "_f")
        P.pool(lambda e: e.memset(tf[:], val), writes=[name + "_f"])
        P.pool(lambda e: e.affine_select(out=tf[:], in_=tf[:], pattern=[[sgn, n]], compare_op=cmp_op,
                                         fill=0.0, base=0, channel_multiplier=-sgn),
               reads=[name + "_f"], writes=[name + "_f"])
        if dt == F32:
            c[name] = tf
            return name + "_f"
        tb = cx.sb([128, n], dt, name)
        P.pool(lambda e: e.tensor_copy(out=tb[:], in_=tf[:]), reads=[name + "_f"], writes=[name])
        c[name] = tb
        return name

    spec = {
        "negge": (ALU.is_ge, BF16, -1.0, True),
        "neglt": (ALU.is_gt, BF16, -1.0),
        "triincl": (ALU.is_ge, F32, 1.0),
        "ustrict": (ALU.is_gt, F32, 1.0),
        "ustrict_b": (ALU.is_gt, BF16, 1.0),
        "mstrict": (ALU.is_gt, F32, 1.0),
        "mincl": (ALU.is_ge, BF16, 1.0),
        "ident_f": (ALU.is_equal, F32, 1.0),
        "ident_b": (ALU.is_equal, BF16, 1.0),
    }
    c["_key"] = {}
    for nm in names:
        if nm in spec:
            c["_key"][nm] = tri(nm, *spec[nm])
        elif nm == "ones_b":
            t = cx.sb([128, 128], BF16, "ones_b")
            P.pool(lambda e, t=t: e.memset(t[:], 1.0), writes=["ones_b"])
            c[nm] = t
            c["_key"][nm] = "ones_b"
        elif nm == "ones_f":
            t = cx.sb([128, 128], F32, "ones_f")
            P.pool(lambda e, t=t: e.memset(t[:], 1.0), writes=["ones_f"])
            c[nm] = t
            c["_key"][nm] = "ones_f"
    return c


def build_A(upto=3):
    import contextlib
    nc = bass.Bass("TRN2", target_bir_lowering=False)
    hT = nc.dram_tensor("hT", [D, L], BF16, kind="ExternalInput").ap()
    wA = nc.dram_tensor("wA", [D, 769], F32, kind="ExternalInput").ap()
    bfg = nc.dram_tensor("bf", [1, 1], F32, kind="ExternalInput").ap()
    yT = nc.dram_tensor("yT", [256, L], BF16, kind="ExternalOutput").ap()
    cscr = nc.dram_tensor("cscr", [6, L], BF16, kind="Internal").ap()
    emit_A(nc, hT, wA, bfg, yT, cscr, upto)
    return nc


def emit_A(nc, hT, wA, bfg, yT, cscr, upto=3):
    import contextlib
    with contextlib.ExitStack() as st:
        cx = Ctx(nc, st)
        P = Prog(nc)
        C = make_consts(P, cx, ["negge", "neglt", "triincl", "ustrict", "mstrict", "mincl",
                                "ident_f", "ones_b", "ones_f"])
        K = C["_key"]
        scale = DH ** -0.5

        QT = [cx.sb([128, L], BF16, "QT%d" % i) for i in range(2)]
        KT = [cx.sb([128, L], BF16, "KT%d" % i) for i in range(2)]
        V = [cx.sb([128, NBLK, 128], BF16, "V%d" % i) for i in range(2)]
        FL = cx.sb([128, NBLK], F32, "FL")
        negb = cx.sb([128, 1], F32, "negb")
        banks = [cx.ps([128, 512], F32, "bank%d" % i) for i in range(8)]
        bk = ["bank%d" % i for i in range(8)]
        st1 = contextlib.ExitStack()
        cx1 = Ctx(nc, st1)
        W = cx1.sb([128, 16, 772], BF16, "W")
        hTt = [cx1.sb([128, 16, 512], BF16, "hTt%d" % i) for i in range(2)]

        wv = wA.rearrange("(c p) n -> p c n", p=128)
        for c4 in range(4):
            P.dma(lambda e, c4=c4: e.dma_start(out=W[:, 4 * c4:4 * c4 + 4, 0:769], in_=wv[:, 4 * c4:4 * c4 + 4, :]),
                  writes=[("W", c4)], q="pool")
        P.dma(lambda e: e.dma_start(out=negb[:], in_=bfg.partition_broadcast(128)), writes=["negb"], q="sp")
        P.dve(lambda e: e.tensor_scalar(out=negb[:], in0=negb[:], scalar1=-1.0, scalar2=None, op0=ALU.mult),
              reads=["negb"], writes=["negb"])
        Wk = [("W", i) for i in range(4)]

        tiles = [(0, 1)] + [(1 + 4 * i, 4) for i in range(16)]
        hv = hT.rearrange("(c p) t -> p c t", p=128)

        rr = 0
        import os
        _nt = int(os.environ.get("A_NT", "17"))
        _dov = int(os.environ.get("A_DOV", "1"))
        _doq = int(os.environ.get("A_DOQ", "1"))
        for ti, (qb0, nb) in enumerate(tiles):
            if ti >= _nt:
                break
            t0 = qb0 * 128
            n = nb * 128
            hb = hTt[ti % 2]
            hk = "hTt%d" % (ti % 2)
            for c4 in range(4):
                P.dma(lambda e, hb=hb, t0=t0, n=n, c4=c4: e.dma_start(
                    out=hb[:, 4 * c4:4 * c4 + 4, 0:n], in_=hv[:, 4 * c4:4 * c4 + 4, t0:t0 + n]),
                    writes=[(hk, c4)], q="sp")
            for which, (dst, col, sc) in enumerate([(QT[0], 0, scale), (KT[0], 128, 1.0),
                                                    (QT[1], 256, scale), (KT[1], 384, 1.0)][:4 * _doq]):
                b = rr % 8
                rr += 1
                for c in range(16):
                    P.pe(lambda e, b=b, c=c, col=col, hb=hb, n=n: e.matmul(
                        banks[b][:, 0:n], W[:, c, col:col + 128], hb[:, c, 0:n],
                        start=(c == 0), stop=(c == 15)),
                        reads=Wk + [(hk, c // 4)], writes=[bk[b]])
                dk = ("QK", which, ti)
                if which % 2 == 0:
                    P.act(lambda e, b=b, dst=dst, t0=t0, n=n, sc=sc: e.activation(
                        out=dst[:, t0:t0 + n], in_=banks[b][:, 0:n], func=AF.Copy, scale=sc),
                        reads=[bk[b]], writes=[dk])
                else:
                    P.dve(lambda e, b=b, dst=dst, t0=t0, n=n: e.tensor_copy(out=dst[:, t0:t0 + n], in_=banks[b][:, 0:n]),
                          reads=[bk[b]], writes=[dk])
            for j in range(nb * _dov):
                blk = qb0 + j
                b = rr % 8
                rr += 1
                for c in range(16):
                    P.pe(lambda e, b=b, c=c, hb=hb, j=j: e.matmul(
                        banks[b][:, 0:257], hb[:, c, j * 128:(j + 1) * 128], W[:, c, 512:769],
                        start=(c == 0), stop=(c == 15)),
                        reads=Wk + [(hk, c // 4)], writes=[bk[b]])
                P.act(lambda e, b=b, blk=blk: e.activation(out=V[0][:, blk, :], in_=banks[b][:, 0:128], func=AF.Copy),
                      reads=[bk[b]], writes=[("V", 0, blk)])
                P.dve(lambda e, b=b, blk=blk: e.tensor_copy(out=V[1][:, blk, :], in_=banks[b][:, 128:256]),
                      reads=[bk[b]], writes=[("V", 1, blk)])
                P.dve(lambda e, b=b, blk=blk: e.tensor_copy(out=FL[:, blk:blk + 1], in_=banks[b][:, 256:257]),
                      reads=[bk[b]], writes=["FL"])

        if upto < 2:
            P.emit()
            st1.close()
            return nc
        P.barrier()
        st1.close()
        spf = cx.sb([128, NBLK], F32, "spf")
        P.act(lambda e: e.activation(out=spf[:], in_=FL[:], func=AF.Exp, bias=negb[:, 0:1], scale=-1.0),
              reads=["FL", "negb"], writes=["spf"])
        P.act(lambda e: e.activation(out=spf[:], in_=spf[:], func=AF.Ln, bias=1.0, scale=1.0),
              reads=["spf"], writes=["spf"])
        P.dve(lambda e: e.memset(spf[0:NPAD, 0:1], 0.0), reads=["spf"], writes=["spf"])
        P.pe(lambda e: e.matmul(banks[0][0:NBLK, 0:128], spf[:, :], C["ones_f"][:, :], start=True, stop=True),
             reads=["spf", K["ones_f"]], writes=[bk[0]])
        abc = cx.sb([NBLK, 128], F32, "abc")
        P.dve(lambda e: e.tensor_copy(out=abc[:], in_=banks[0][0:NBLK, 0:128]), reads=[bk[0]], writes=["abc"])
        P.pe(lambda e: e.matmul(banks[1][0:NBLK, 0:128], C["ustrict"][0:NBLK, 0:NBLK], abc[:, :],
                                start=True, stop=False), reads=["abc", K["ustrict"]], writes=[bk[1]])
        P.pe(lambda e: e.matmul(banks[1][0:NBLK, 0:128], spf[:, :], C["triincl"][:, :],
                                start=False, stop=True), reads=["spf", K["triincl"]], writes=[bk[1]])
        cp = cx.sb([NBLK, 128], F32, "cp")
        P.dve(lambda e: e.tensor_copy(out=cp[:], in_=banks[1][0:NBLK, 0:128]), reads=[bk[1]], writes=["cp"])
        parts_p = cx.sb([NBLK, 3, 128], BF16, "parts_p")
        parts_n = cx.sb([NBLK, 3, 128], BF16, "parts_n")
        tmpf = cx.sb([NBLK, 128], F32, "tmpf")
        for k in range(3):
            P.dve(lambda e, k=k: e.tensor_copy(out=parts_p[:, k, :], in_=cp[:]), reads=["cp"], writes=["parts_p"])
            P.dve(lambda e, k=k: e.tensor_copy(out=tmpf[:], in_=parts_p[:, k, :]), reads=["parts_p"], writes=["tmpf"])
            P.dve(lambda e, k=k: e.tensor_sub(out=cp[:], in0=cp[:], in1=tmpf[:]), reads=["cp", "tmpf"], writes=["cp"])
        P.dve(lambda e: e.tensor_scalar(out=parts_n[:], in0=parts_p[:], scalar1=-1.0, scalar2=None, op0=ALU.mult),
              reads=["parts_p"], writes=["parts_n"])
        P.dve(lambda e: e.memset(parts_p[0:1, 0, 0:NPAD], -30000.0), reads=["parts_p", "parts_n"], writes=["parts_p"])
        LK = cx.sb([6, L], BF16, "LK")
        RQ = cx.sb([6, L], BF16, "RQ")
        P.pool(lambda e: e.memset(LK[:], 1.0), writes=["LK"])
        P.pool(lambda e: e.memset(RQ[:], 1.0), writes=["RQ"])
        P.dma(lambda e: e.dma_start(out=cscr[0:3, :].rearrange("k (b p) -> b k p", p=128), in_=parts_p[:]),
              reads=["parts_p"], writes=["cscr_p"], q="sp")
        P.dma(lambda e: e.dma_start(out=cscr[3:6, :].rearrange("k (b p) -> b k p", p=128), in_=parts_n[:]),
              reads=["parts_n"], writes=["cscr_n"], q="sp")
        P.dma(lambda e: e.dma_start(out=LK[3:6, :], in_=cscr[0:3, :]), reads=["cscr_p", "LK"], writes=["LK"], q="sp")
        P.dma(lambda e: e.dma_start(out=RQ[0:3, :], in_=cscr[3:6, :]), reads=["cscr_n", "RQ"], writes=["RQ"], q="sp")

        if upto < 3:
            P.emit()
            return nc
        e_t = [cx.sb([128, 512], F32, "e%d" % i) for i in range(2)]
        sp_t = [cx.sb([128, 512], BF16, "sp%d" % i) for i in range(3)]
        g_t = [cx.sb([128, 512], F32, "g%d" % i) for i in range(2)]
        w_t = [cx.sb([128, 512], BF16, "w%d" % i) for i in range(2)]
        p_t = [cx.sb([128, 512], BF16, "pp%d" % i) for i in range(2)]
        yst = [cx.sb([128, 512], BF16, "yst%d" % i) for i in range(4)]
        rec = cx.sb([128, 512], F32, "rec")
        ZB = [0, 1]
        AB, OB = 2, 3
        SB_ = [4, 5]
        O2, DB = 6, 7

        def do_tile(ti, qb0, nb):
            t0 = qb0 * 128
            n = nb * 128
            nsteps = qb0 + nb

            def kkeys(jb):
                tj = 0 if jb == 0 else 1 + (jb - 1) // 4
                return tj

            P.dve(lambda e, n=n: e.memset(banks[AB][:, 0:n], 0.0), writes=[bk[AB]])
            P.dve(lambda e, n=n: e.memset(banks[OB][:, 0:n], 0.0), writes=[bk[OB]])
            P.dve(lambda e, n=n: e.memset(banks[O2][:, 0:n], 0.0), writes=[bk[O2]])
            P.dve(lambda e, n=n: e.memset(banks[DB][:, 0:n], 0.0), writes=[bk[DB]])

            def cols(i):
                jb = qb0 + nb - 1 - i
                r = jb - qb0
                c0 = max(r, 0) * 128
                return jb, r, c0

            def sbA(i):
                jb, r, c0 = cols(i)
                zb = ZB[i % 2]
                eb = e_t[i % 2]
                ek = "e%d" % (i % 2)
                spb = sp_t[i % 3]
                spk = "sp%d" % (i % 3)
                tj = kkeys(jb)
                P.pe(lambda e: e.matmul(banks[zb][:, c0:n], KT[0][:, jb * 128:(jb + 1) * 128], QT[0][:, t0 + c0:t0 + n],
                                        start=True, stop=True),
                     reads=[("QK", 1, tj), ("QK", 0, ti)], writes=[bk[zb]])
                P.act(lambda e: e.activation(out=eb[:, c0:n], in_=banks[zb][:, c0:n], func=AF.Exp),
                      reads=[bk[zb]], writes=[ek])
                if r >= 0:
                    P.dve(lambda e: e.tensor_mul(out=eb[:, c0:c0 + 128], in0=eb[:, c0:c0 + 128], in1=C["mstrict"][:]),
                          reads=[ek, K["mstrict"]], writes=[ek])
                if jb == 0:
                    P.dve(lambda e: e.memset(eb[0:NPAD, c0:n], 0.0), reads=[ek], writes=[ek])
                P.act(lambda e: e.activation(out=spb[:, c0:n], in_=eb[:, c0:n], func=AF.Ln, bias=1.0, scale=1.0),
                      reads=[ek], writes=[spk])

            def sbB(i):
                jb, r, c0 = cols(i)
                spb = sp_t[i % 3]
                spk = "sp%d" % (i % 3)
                eb = e_t[i % 2]
                ek = "e%d" % (i % 2)
                gb = g_t[i % 2]
                gk = "g%d" % (i % 2)
                wb = w_t[i % 2]
                wk = "w%d" % (i % 2)
                if i > 0:
                    _, _, pc0 = cols(i - 1)
                    spp = sp_t[(i - 1) % 3]
                    sppk = "sp%d" % ((i - 1) % 3)
                    P.pe(lambda e: e.matmul(banks[AB][:, pc0:n], C["neglt"][:], spp[:, pc0:n],
                                            start=False, stop=True, skip_group_check=True),
                         reads=[sppk, K["neglt"]], writes=[bk[AB]])
                P.pe(lambda e: e.matmul(banks[AB][:, c0:n], C["negge"][:], spb[:, c0:n],
                                        start=False, stop=True, skip_group_check=True),
                     reads=[spk, K["negge"]], writes=[bk[AB]])
                P.act(lambda e: e.activation(out=gb[:, c0:n], in_=banks[AB][:, c0:n], func=AF.Exp),
                      reads=[bk[AB]], writes=[gk])
                P.dve(lambda e: e.tensor_mul(out=wb[:, c0:n], in0=eb[:, c0:n], in1=gb[:, c0:n]),
                      reads=[ek, gk], writes=[wk])

            def sbC(i):
                jb, r, c0 = cols(i)
                wb = w_t[i % 2]
                wk = "w%d" % (i % 2)
                P.pe(lambda e: e.matmul(banks[OB][:, c0:n], V[0][:, jb, :], wb[:, c0:n],
                                        start=False, stop=True, skip_group_check=True),
                     reads=[wk, ("V", 0, jb)], writes=[bk[OB]])

            def fxA(i):
                jb, r, c0 = cols(i)
                sb = SB_[i % 2]
                pb = p_t[i % 2]
                pk = "pp%d" % (i % 2)
                tj = kkeys(jb)
                P.pe(lambda e: e.matmul(banks[sb][:, c0:n], KT[1][:, jb * 128:(jb + 1) * 128], QT[1][:, t0 + c0:t0 + n],
                                        start=True, stop=False),
                     reads=[("QK", 3, tj), ("QK", 2, ti)], writes=[bk[sb]])
                P.pe(lambda e: e.matmul(banks[sb][:, c0:n], LK[0:6, jb * 128:(jb + 1) * 128], RQ[0:6, t0 + c0:t0 + n],
                                        start=False, stop=True),
                     reads=["LK", "RQ"], writes=[bk[sb]])
                P.act(lambda e: e.activation(out=pb[:, c0:n], in_=banks[sb][:, c0:n], func=AF.Exp),
                      reads=[bk[sb]], writes=[pk])
                if r >= 0:
                    P.dve(lambda e: e.tensor_mul(out=pb[:, c0:c0 + 128], in0=pb[:, c0:c0 + 128], in1=C["mincl"][:]),
                          reads=[pk, K["mincl"]], writes=[pk])

            def fxB(i):
                jb, r, c0 = cols(i)
                pb = p_t[i % 2]
                pk = "pp%d" % (i % 2)
                P.pe(lambda e: e.matmul(banks[O2][:, c0:n], V[1][:, jb, :], pb[:, c0:n],
                                        start=False, stop=True, skip_group_check=True),
                     reads=[pk, ("V", 1, jb)], writes=[bk[O2]])
                P.pe(lambda e: e.matmul(banks[DB][:, c0:n], C["ones_b"][:], pb[:, c0:n],
                                        start=False, stop=True, skip_group_check=True),
                     reads=[pk, K["ones_b"]], writes=[bk[DB]])

            for it in range(nsteps + 2):
                if it < nsteps:
                    sbA(it)
                    fxA(it)
                if 0 <= it - 1 < nsteps:
                    sbB(it - 1)
                    fxB(it - 1)
                if 0 <= it - 2 < nsteps:
                    sbC(it - 2)

            ys = yst[(2 * ti) % 4]
            ysk = "yst%d" % ((2 * ti) % 4)
            yf = yst[(2 * ti + 1) % 4]
            yfk = "yst%d" % ((2 * ti + 1) % 4)
            P.act(lambda e, ys=ys, n=n: e.activation(out=ys[:, 0:n], in_=banks[OB][:, 0:n], func=AF.Copy),
                  reads=[bk[OB]], writes=[ysk])
            P.dma(lambda e, ys=ys, n=n, t0=t0: e.dma_start(out=yT[0:128, t0:t0 + n], in_=ys[:, 0:n]),
                  reads=[ysk], writes=[("yT", 0, ti)], q="sp")
            P.dve(lambda e, n=n: e.tensor_scalar(out=rec[:, 0:n], in0=banks[DB][:, 0:n], scalar1=1e-30, scalar2=None,
                                                 op0=ALU.add), reads=[bk[DB]], writes=["rec"])
            P.dve(lambda e, n=n: e.reciprocal(out=rec[:, 0:n], in_=rec[:, 0:n]), reads=["rec"], writes=["rec"])
            P.dve(lambda e, yf=yf, n=n: e.tensor_mul(out=yf[:, 0:n], in0=banks[O2][:, 0:n], in1=rec[:, 0:n]),
                  reads=[bk[O2], "rec"], writes=[yfk])
            P.dma(lambda e, yf=yf, n=n, t0=t0: e.dma_start(out=yT[128:256, t0:t0 + n], in_=yf[:, 0:n]),
                  reads=[yfk], writes=[("yT", 1, ti)], q="sp")

        for ti, (qb0, nb) in enumerate(tiles):
            do_tile(ti, qb0, nb)
        P.emit()
    return nc


def build_T(first):
    nc = bass.Bass("TRN2", target_bir_lowering=False)
    io = {}
    def din(name, shape, dt=F32):
        io[name] = nc.dram_tensor(name, list(shape), dt, kind="ExternalInput").ap()
    if first:
        din("xin", [NT, D]); din("lng", [2, D])
    else:
        din("hprev", [NT, D]); din("hTp", [D, NT], BF16); din("yT", [D, NT], BF16)
        din("wg", [D, 2 * D]); din("wbs", [1024, D]); din("wbf", [1024, D]); din("wo", [D, D])
        din("lnm", [2, D]); din("wr", [D, NE]); din("rb", [1, NE])
        import os
        _ne = int(os.environ.get("T_NEXP", NE))
        din("wgate", [_ne, D, FF]); din("wup", [_ne, D, FF]); din("wdown", [_ne, FF, D]); din("lnf", [2, D])
    io["h"] = nc.dram_tensor("h", [NT, D], F32, kind="ExternalOutput").ap()
    io["hT"] = nc.dram_tensor("hT", [D, NT], BF16, kind="ExternalOutput").ap()
    emit_T(nc, io, first)
    return nc


def emit_T(nc, io, first):
    import contextlib
    with contextlib.ExitStack() as st:
        cx = Ctx(nc, st)
        P = Prog(nc)
        C = make_consts(P, cx, ["ident_f", "ident_b", "ustrict_b", "ones_b"])
        K = C["_key"]
        fb = [cx.ps([128, 512], F32, "bankf%d" % i) for i in range(6)]
        fk = ["bankf%d" % i for i in range(6)]
        tb = [cx.ps([128, 1024], BF16, "bankt%d" % i) for i in range(2)]
        tk = ["bankt%d" % i for i in range(2)]
        rr = [0]

        def nbank():
            rr[0] += 1
            return rr[0] % 6

        X = cx.sb([128, TB, D], F32, "X")
        XK = lambda b: ("X", b)
        st_s = cx.sb([128, 8], F32, "st_s")

        lncnt = [0]

        def layer_norm(gb_ap, cxl):
            lncnt[0] += 1
            G = cxl.sb([128, D], F32, "lnG%d" % lncnt[0])
            Bt = cxl.sb([128, D], F32, "lnB%d" % lncnt[0])
            junk = cxl.sb([128, D], BF16, "lnjunk%d" % lncnt[0])
            P.dma(lambda e: e.dma_start(out=G[:], in_=gb_ap[0:1, :].partition_broadcast(128)), writes=["lnG"])
            P.dma(lambda e: e.dma_start(out=Bt[:], in_=gb_ap[1:2, :].partition_broadcast(128)), writes=["lnB"])
            for b in range(TB):
                xs = X[:, b, :]
                P.dve(lambda e, xs=xs: e.reduce_sum(out=st_s[:, 0:1], in_=xs, axis=AX.X), reads=[XK(b)], writes=["st_s"])
                P.dve(lambda e: e.memset(st_s[:, 1:2], 0.0), writes=["st_s1"])
                P.act(lambda e, xs=xs: e.activation(out=junk[:], in_=xs, func=AF.Square, accum_out=st_s[:, 1:2]),
                      reads=[XK(b), "st_s1"], writes=["st_s1", "lnjunk"])
                P.dve(lambda e: e.tensor_scalar(out=st_s[:, 2:3], in0=st_s[:, 0:1], scalar1=1.0 / D, scalar2=None,
                                                op0=ALU.mult), reads=["st_s"], writes=["st_s2"])
                P.dve(lambda e: e.tensor_mul(out=st_s[:, 3:4], in0=st_s[:, 2:3], in1=st_s[:, 2:3]),
                      reads=["st_s2"], writes=["st_s3"])
                P.dve(lambda e: e.scalar_tensor_tensor(out=st_s[:, 4:5], in0=st_s[:, 1:2], scalar=1.0 / D,
                                                       in1=st_s[:, 3:4], op0=ALU.mult, op1=ALU.subtract),
                      reads=["st_s1", "st_s3"], writes=["st_s4"])
                P.act(lambda e: e.activation(out=st_s[:, 5:6], in_=st_s[:, 4:5], func=AF.Sqrt, bias=EPS_T[:, 0:1], scale=1.0),
                      reads=["st_s4", "eps"], writes=["st_s5"])
                P.dve(lambda e: e.reciprocal(out=st_s[:, 6:7], in_=st_s[:, 5:6]), reads=["st_s5"], writes=["st_s6"])
                P.dve(lambda e, xs=xs: e.tensor_scalar(out=xs, in0=xs, scalar1=st_s[:, 2:3], scalar2=st_s[:, 6:7],
                                                       op0=ALU.subtract, op1=ALU.mult),
                      reads=[XK(b), "st_s2", "st_s6"], writes=[XK(b)])
                P.dve(lambda e, xs=xs: e.tensor_mul(out=xs, in0=xs, in1=G[:]), reads=[XK(b), "lnG"], writes=[XK(b)])
                P.pool(lambda e, xs=xs: e.tensor_add(out=xs, in0=xs, in1=Bt[:]), reads=[XK(b), "lnB"], writes=[XK(b)])

        EPS_T = cx.sb([128, 1], F32, "eps")
        P.pool(lambda e: e.memset(EPS_T[:], EPS), writes=["eps"])

        def write_outputs(Hb, cxl):
            hv = io["h"].rearrange("(b p) d -> p b d", p=128)
            hTv = io["hT"].rearrange("(c p) t -> p c t", p=128)
            hts = [cxl.sb([128, 16, 128], BF16, "hts%d" % i) for i in range(2)]
            for b in range(TB):
                P.dma(lambda e, b=b: e.dma_start(out=hv[:, b, :], in_=X[:, b, :]), reads=[XK(b)], writes=[("hout", b)])
                P.act(lambda e, b=b: e.activation(out=Hb[:, b, :], in_=X[:, b, :], func=AF.Copy),
                      reads=[XK(b)], writes=[("Hb", b)])
                hs = hts[b % 2]
                hk = "hts%d" % (b % 2)
                for half in range(2):
                    t = tb[half]
                    for c in range(8):
                        cc = half * 8 + c
                        P.pe(lambda e, t=t, c=c, cc=cc, b=b: e.transpose(t[:, c * 128:(c + 1) * 128],
                                                                       Hb[:, b, cc * 128:(cc + 1) * 128], C["ident_b"][:]),
                             reads=[("Hb", b), K["ident_b"]], writes=[tk[half]])
                    if half == 0:
                        P.dve(lambda e, t=t, hs=hs: e.tensor_copy(out=hs[:, 0:8, :], in_=t[:].rearrange("p (c t) -> p c t", t=128)),
                              reads=[tk[half]], writes=[(hk, 0)])
                    else:
                        P.act(lambda e, t=t, hs=hs: e.activation(out=hs[:, 8:16, :], in_=t[:].rearrange("p (c t) -> p c t", t=128),
                                                                func=AF.Copy), reads=[tk[half]], writes=[(hk, 1)])
                P.dma(lambda e, b=b, hs=hs: e.dma_start(out=hTv[:, :, b * 128:(b + 1) * 128], in_=hs[:]),
                      reads=[(hk, 0), (hk, 1)], writes=[("hTout", b)])

        if first:
            xv = io["xin"].rearrange("(b p) d -> p b d", p=128)
            for b in range(TB):
                P.dma(lambda e, b=b: e.dma_start(out=X[:, b, :], in_=xv[:, b, :]), writes=[XK(b)])
            Hb = cx.sb([128, TB, D], BF16, "Hb")
            layer_norm(io["lng"], cx)
            write_outputs(Hb, cx)
            P.emit()
            return

        stA = contextlib.ExitStack()
        cxA = Ctx(nc, stA)
        MT = cxA.sb([128, 16, NT], BF16, "MT")
        stA1 = contextlib.ExitStack()
        cxA1 = Ctx(nc, stA1)
        Xb = X[:].rearrange("p b d -> p (b d)").bitcast(BF16)
        hTp = Xb[:, 0:16 * NT].rearrange("p (c t) -> p c t", t=NT)
        yTs = Xb[:, 16 * NT:32 * NT].rearrange("p (c t) -> p c t", t=NT)
        hv_ = io["hTp"].rearrange("(c p) t -> p c t", p=128)
        yv_ = io["yT"].rearrange("(c p) t -> p c t", p=128)
        for c4 in range(4):
            P.dma(lambda e, c4=c4: e.dma_start(out=hTp[:, 4 * c4:4 * c4 + 4, :], in_=hv_[:, 4 * c4:4 * c4 + 4, :]),
                  writes=[("hTp", c4)])
            P.dma(lambda e, c4=c4: e.dma_start(out=yTs[:, 4 * c4:4 * c4 + 4, :], in_=yv_[:, 4 * c4:4 * c4 + 4, :]),
                  writes=[("yTs", c4)])
        wgs = [cxA1.sb([128, 16, 128], BF16, "wgs%d" % i) for i in range(2)]
        wgf = [cxA1.sb([128, 16, 128], BF16, "wgf%d" % i) for i in range(2)]
        wbs = [cxA1.sb([128, 8, 128], BF16, "wbs%d" % i) for i in range(2)]
        wbf = [cxA1.sb([128, 8, 128], BF16, "wbf%d" % i) for i in range(2)]
        sgs = [cxA1.sb([128, 384], F32, "sgs%d" % i) for i in range(2)]
        sgf = [cxA1.sb([128, 384], F32, "sgf%d" % i) for i in range(2)]
        m1 = [cxA1.sb([128, 384], F32, "m1_%d" % i) for i in range(2)]
        m2 = [cxA1.sb([128, 384], F32, "m2_%d" % i) for i in range(2)]
        wgv = io["wg"].rearrange("(c p) n -> p c n", p=128)
        wbsv = io["wbs"].rearrange("(c p) n -> p c n", p=128)
        wbfv = io["wbf"].rearrange("(c p) n -> p c n", p=128)
        it = 0
        for nci in range(16):
            w = nci % 2
            n0 = nci * 128
            for hf in range(2):
                P.dma(lambda e, w=w, n0=n0, hf=hf: e.dma_start(out=wgs[w][:, 8 * hf:8 * hf + 8, :],
                                                              in_=wgv[:, 8 * hf:8 * hf + 8, n0:n0 + 128]),
                      writes=[("wgs", w, hf)], q="pool")
                P.dma(lambda e, w=w, n0=n0, hf=hf: e.dma_start(out=wgf[w][:, 8 * hf:8 * hf + 8, :],
                                                              in_=wgv[:, 8 * hf:8 * hf + 8, D + n0:D + n0 + 128]),
                      writes=[("wgf", w, hf)], q="pool")
            P.dma(lambda e, w=w, n0=n0: e.dma_start(out=wbs[w][:], in_=wbsv[:, :, n0:n0 + 128]), writes=[("wbs", w)], q="pool")
            P.dma(lambda e, w=w, n0=n0: e.dma_start(out=wbf[w][:], in_=wbfv[:, :, n0:n0 + 128]), writes=[("wbf", w)], q="pool")
            for tr in range(3):
                ts = slice(tr * 384, (tr + 1) * 384)
                u = it % 2
                it += 1
                bg, bb_, bg2, bb2 = nbank(), nbank(), nbank(), nbank()
                for c in range(16):
                    P.pe(lambda e, c=c, w=w, ts=ts, bg=bg: e.matmul(fb[bg][:, 0:384], wgs[w][:, c, :], hTp[:, c, ts],
                                                                    start=(c == 0), stop=(c == 15)),
                         reads=[("wgs", w, c // 8), ("hTp", c // 4)], writes=[fk[bg]])
                for c in range(8):
                    P.pe(lambda e, c=c, w=w, ts=ts, bb_=bb_: e.matmul(fb[bb_][:, 0:384], wbs[w][:, c, :], yTs[:, c, ts],
                                                                      start=(c == 0), stop=(c == 7)),
                         reads=[("wbs", w), ("yTs", c // 4)], writes=[fk[bb_]])
                for c in range(16):
                    P.pe(lambda e, c=c, w=w, ts=ts, bg2=bg2: e.matmul(fb[bg2][:, 0:384], wgf[w][:, c, :], hTp[:, c, ts],
                                                                      start=(c == 0), stop=(c == 15)),
                         reads=[("wgf", w, c // 8), ("hTp", c // 4)], writes=[fk[bg2]])
                for c in range(8):
                    P.pe(lambda e, c=c, w=w, ts=ts, bb2=bb2: e.matmul(fb[bb2][:, 0:384], wbf[w][:, c, :], yTs[:, 8 + c, ts],
                                                                      start=(c == 0), stop=(c == 7)),
                         reads=[("wbf", w), ("yTs", 2 + c // 4)], writes=[fk[bb2]])
                P.act(lambda e, u=u, bg=bg: e.activation(out=sgs[u][:], in_=fb[bg][:, 0:384], func=AF.Sigmoid),
                      reads=[fk[bg]], writes=[("sgs", u)])
                P.act(lambda e, u=u, bg2=bg2: e.activation(out=sgf[u][:], in_=fb[bg2][:, 0:384], func=AF.Sigmoid),
                      reads=[fk[bg2]], writes=[("sgf", u)])
                P.dve(lambda e, u=u, bb_=bb_: e.tensor_mul(out=m1[u][:], in0=sgs[u][:], in1=fb[bb_][:, 0:384]),
                      reads=[("sgs", u), fk[bb_]], writes=[("m1", u)])
                P.dve(lambda e, u=u, bb2=bb2: e.tensor_mul(out=m2[u][:], in0=sgf[u][:], in1=fb[bb2][:, 0:384]),
                      reads=[("sgf", u), fk[bb2]], writes=[("m2", u)])
                P.pool(lambda e, u=u, nci=nci, ts=ts: e.tensor_add(out=MT[:, nci, ts], in0=m1[u][:], in1=m2[u][:]),
                       reads=[("m1", u), ("m2", u)], writes=[("MT", nci)])
        import os
        _stop = int(os.environ.get("T_STOP", "9"))
        if _stop <= 1:
            P.emit(); stA1.close(); stA.close(); return
        P.barrier()
        stA1.close()

        stA2 = contextlib.ExitStack()
        cxA2 = Ctx(nc, stA2)
        hpv = io["hprev"].rearrange("(b p) d -> p b d", p=128)
        for b in range(TB):
            P.dma(lambda e, b=b: e.dma_start(out=X[:, b, :], in_=hpv[:, b, :]), writes=[XK(b)])
        woc = [cxA2.sb([128, 16, 512], BF16, "woc%d" % i) for i in range(2)]
        wov = io["wo"].rearrange("(c p) n -> p c n", p=128)
        for cg in range(4):
            w = cg % 2
            for q4 in range(4):
                P.dma(lambda e, w=w, cg=cg, q4=q4: e.dma_start(out=woc[w][:, 4 * q4:4 * q4 + 4, :],
                                                              in_=wov[:, 4 * q4:4 * q4 + 4, cg * 512:(cg + 1) * 512]),
                      writes=[("woc", w, q4)], q="pool")
            for b in range(TB):
                bo = nbank()
                for c in range(16):
                    P.pe(lambda e, c=c, w=w, b=b, bo=bo: e.matmul(fb[bo][:, :], MT[:, c, b * 128:(b + 1) * 128], woc[w][:, c, :],
                                                                  start=(c == 0), stop=(c == 15)),
                         reads=[("MT", c), ("woc", w, c // 4)], writes=[fk[bo]])
                P.dve(lambda e, b=b, cg=cg, bo=bo: e.scalar_tensor_tensor(
                    out=X[:, b, cg * 512:(cg + 1) * 512], in0=X[:, b, cg * 512:(cg + 1) * 512], scalar=ALPHA,
                    in1=fb[bo][:, :], op0=ALU.mult, op1=ALU.add), reads=[XK(b), fk[bo]], writes=[XK(b)])
        layer_norm(io["lnm"], cxA2)
        if _stop <= 2:
            P.emit(); stA2.close(); stA.close(); return
        P.barrier()
        stA2.close()
        stA.close()

        Hb = cx.sb([128, TB, D], BF16, "Hb")
        RT = cx.sb([128, TB, NE], F32, "aff")
        MSK = cx.sb([128, TB, NE], F32, "msk")
        WN = cx.sb([128, TB, NE], F32, "wn")
        POS = cx.sb([128, TB, NE], F32, "pos")
        WHL = cx.sb([128, TB, NE, 2], BF16, "whl")
        iota_f = cx.sb([128, CAP], F32, "iota_f")
        iota_i = cx.sb([128, CAP], I32, "iota_i")
        P.pool(lambda e: e.iota(iota_i[:], pattern=[[1, CAP]], base=0, channel_multiplier=0), writes=["iota_i"])
        P.pool(lambda e: e.tensor_copy(out=iota_f[:], in_=iota_i[:]), reads=["iota_i"], writes=["iota_f"])
        stC = contextlib.ExitStack()
        cxC = Ctx(nc, stC)
        wr_sb = cxC.sb([128, 16, NE], F32, "wr_sb")
        rb_sb = cxC.sb([128, NE], F32, "rb_sb")
        wrv = io["wr"].rearrange("(c p) e -> p c e", p=128)
        for c4 in range(4):
            P.dma(lambda e, c4=c4: e.dma_start(out=wr_sb[:, 4 * c4:4 * c4 + 4, :], in_=wrv[:, 4 * c4:4 * c4 + 4, :]),
                  writes=[("wr", c4)])
        P.dma(lambda e: e.dma_start(out=rb_sb[:], in_=io["rb"].partition_broadcast(128)), writes=["rb"])
        xT32 = [cxC.sb([128, 16, 128], F32, "xT32_%d" % i) for i in range(2)]
        for b in range(TB):
            P.act(lambda e, b=b: e.activation(out=Hb[:, b, :], in_=X[:, b, :], func=AF.Copy), reads=[XK(b)], writes=[("Hb", b)])
            xt = xT32[b % 2]
            xk = "xT32_%d" % (b % 2)
            for q4 in range(4):
                bt_ = nbank()
                for c in range(4):
                    cc = q4 * 4 + c
                    P.pe(lambda e, b=b, c=c, cc=cc, bt_=bt_: e.transpose(fb[bt_][:, c * 128:(c + 1) * 128],
                                                                        X[:, b, cc * 128:(cc + 1) * 128], C["ident_f"][:]),
                         reads=[XK(b), K["ident_f"]], writes=[fk[bt_]])
                if q4 % 2 == 0:
                    P.dve(lambda e, xt=xt, q4=q4, bt_=bt_: e.tensor_copy(out=xt[:, 4 * q4:4 * q4 + 4, :],
                                                                        in_=fb[bt_][:].rearrange("p (c t) -> p c t", t=128)),
                          reads=[fk[bt_]], writes=[(xk, q4)])
                else:
                    P.act(lambda e, xt=xt, q4=q4, bt_=bt_: e.activation(out=xt[:, 4 * q4:4 * q4 + 4, :],
                                                                       in_=fb[bt_][:].rearrange("p (c t) -> p c t", t=128),
                                                                       func=AF.Copy), reads=[fk[bt_]], writes=[(xk, q4)])
            br = nbank()
            for c in range(16):
                P.pe(lambda e, c=c, xt=xt, br=br: e.matmul(fb[br][:, 0:NE], xt[:, c, :], wr_sb[:, c, :],
                                                           start=(c == 0), stop=(c == 15)),
                     reads=[(xk, c // 4), ("wr", c // 4)], writes=[fk[br]])
            P.act(lambda e, b=b, br=br: e.activation(out=RT[:, b, :], in_=fb[br][:, 0:NE], func=AF.Sigmoid),
                  reads=[fk[br]], writes=[("aff", b)])
        for b in range(TB):
            P.pool(lambda e, b=b: e.tensor_scalar(out=X[:, b, :], in0=X[:, b, :], scalar1=ALPHA, scalar2=None, op0=ALU.mult),
                   reads=[XK(b)], writes=[XK(b)])
        affk = [("aff", b) for b in range(TB)]
        sel = cxC.sb([128, TB, NE], F32, "sel")
        sel2 = cxC.sb([128, TB, NE], F32, "sel2")
        eq = cxC.sb([128, TB, NE], F32, "eq")
        mx1 = cxC.sb([128, TB * 4], F32, "mx1")
        mx2 = cxC.sb([128, TB * 4], F32, "mx2")
        gs = cxC.sb([128, TB, 4], F32, "gs")
        gmax = cxC.sb([128, TB], F32, "gmax")
        best = cxC.sb([128, TB * 4], F32, "best")
        wsum = cxC.sb([128, TB], F32, "wsum")
        tmpw = cxC.sb([128, TB, NE], F32, "tmpw")
        for b in range(TB):
            P.dve(lambda e, b=b: e.tensor_add(out=sel[:, b, :], in0=RT[:, b, :], in1=rb_sb[:]),
                  reads=[("aff", b), "rb"], writes=["sel"])
        s4 = lambda t: t[:].rearrange("p b (g k) -> p (b g) k", k=4)
        bc = lambda t: t[:].unsqueeze(2).to_broadcast([128, TB * 4, 4])
        P.dve(lambda e: e.tensor_reduce(out=mx1[:], in_=s4(sel), axis=AX.X, op=ALU.max), reads=["sel"], writes=["mx1"])
        P.dve(lambda e: e.tensor_tensor(out=s4(eq), in0=s4(sel), in1=bc(mx1), op=ALU.is_equal),
              reads=["sel", "mx1"], writes=["eq"])
        P.dve(lambda e: e.scalar_tensor_tensor(out=sel2[:], in0=eq[:], scalar=-1e9, in1=sel[:], op0=ALU.mult, op1=ALU.add),
              reads=["eq", "sel"], writes=["sel2"])
        P.dve(lambda e: e.tensor_reduce(out=mx2[:], in_=s4(sel2), axis=AX.X, op=ALU.max), reads=["sel2"], writes=["mx2"])
        P.dve(lambda e: e.tensor_add(out=gs[:].rearrange("p b g -> p (b g)"), in0=mx1[:], in1=mx2[:]),
              reads=["mx1", "mx2"], writes=["gs"])
        P.dve(lambda e: e.tensor_reduce(out=gmax[:], in_=gs[:], axis=AX.X, op=ALU.max), reads=["gs"], writes=["gmax"])
        P.dve(lambda e: e.tensor_tensor(out=best[:].rearrange("p (b g) -> p b g", g=4), in0=gs[:],
                                        in1=gmax[:].unsqueeze(2).to_broadcast([128, TB, 4]), op=ALU.is_equal),
              reads=["gs", "gmax"], writes=["best"])
        P.dve(lambda e: e.tensor_tensor(out=s4(eq), in0=s4(sel), in1=bc(mx2), op=ALU.is_ge),
              reads=["sel", "mx2", "sel2"], writes=["eq"])
        P.dve(lambda e: e.tensor_tensor(out=s4(MSK), in0=s4(eq), in1=bc(best), op=ALU.mult),
              reads=["eq", "best"], writes=["msk"])
        P.dve(lambda e: e.memset(MSK[0:NPAD, 0, :], 0.0), reads=["msk"], writes=["msk"])
        P.dve(lambda e: e.tensor_mul(out=tmpw[:], in0=MSK[:], in1=RT[:]), reads=["msk"] + affk, writes=["tmpw"])
        P.dve(lambda e: e.tensor_reduce(out=wsum[:], in_=tmpw[:], axis=AX.X, op=ALU.add), reads=["tmpw"], writes=["wsum"])
        P.dve(lambda e: e.tensor_scalar(out=wsum[:], in0=wsum[:], scalar1=1e-30, scalar2=None, op0=ALU.add),
              reads=["wsum"], writes=["wsum"])
        P.dve(lambda e: e.reciprocal(out=wsum[:], in_=wsum[:]), reads=["wsum"], writes=["wsum"])
        P.dve(lambda e: e.tensor_tensor(out=WN[:], in0=tmpw[:], in1=wsum[:].unsqueeze(2).to_broadcast([128, TB, NE]),
                                        op=ALU.mult), reads=["tmpw", "wsum"], writes=["wn"])
        P.dve(lambda e: e.tensor_copy(out=WHL[:, :, :, 0], in_=WN[:]), reads=["wn"], writes=["whl0"])
        P.dve(lambda e: e.tensor_copy(out=tmpw[:], in_=WHL[:, :, :, 0]), reads=["whl0", "wn"], writes=["tmpw"])
        P.dve(lambda e: e.tensor_sub(out=tmpw[:], in0=WN[:], in1=tmpw[:]), reads=["wn", "tmpw"], writes=["tmpw"])
        P.dve(lambda e: e.tensor_copy(out=WHL[:, :, :, 1], in_=tmpw[:]), reads=["tmpw"], writes=["whl1"])
        mskb = cxC.sb([128, TB, NE], BF16, "mskb")
        cmb = cxC.sb([128, TB, NE], BF16, "cmb")
        P.dve(lambda e: e.tensor_copy(out=mskb[:], in_=MSK[:]), reads=["msk"], writes=["mskb"])
        P.dve(lambda e: e.tensor_copy(out=cmb[:, 0, :], in_=mskb[:, 0, :]), reads=["mskb"], writes=[("cmb", 0)])
        for b in range(1, TB):
            P.dve(lambda e, b=b: e.tensor_add(out=cmb[:, b, :], in0=cmb[:, b - 1, :], in1=mskb[:, b, :]),
                  reads=["mskb", ("cmb", b - 1)], writes=[("cmb", b)])
        for b in range(TB):
            bp = nbank()
            P.pe(lambda e, b=b, bp=bp: e.matmul(fb[bp][:, 0:NE], C["ustrict_b"][:], mskb[:, b, :], start=True, stop=(b == 0)),
                 reads=["mskb", K["ustrict_b"]], writes=[fk[bp]])
            if b > 0:
                P.pe(lambda e, b=b, bp=bp: e.matmul(fb[bp][:, 0:NE], C["ones_b"][:], cmb[:, b - 1, :], start=False, stop=True),
                     reads=[("cmb", b - 1), K["ones_b"]], writes=[fk[bp]])
            P.dve(lambda e, b=b, bp=bp: e.tensor_copy(out=POS[:, b, :], in_=fb[bp][:, 0:NE]), reads=[fk[bp]], writes=["pos"])
        if _stop <= 3:
            P.emit(); stC.close(); return
        P.barrier()
        stC.close()

        stD = contextlib.ExitStack()
        cxD = Ctx(nc, stD)
        S = [cxD.sb([128, TB, CAP], BF16, "S%d" % i) for i in range(2)]
        ST = [cxD.sb([128, 2, NT], BF16, "ST%d" % i) for i in range(2)]
        XeT = cxD.sb([128, 16, CAP], BF16, "XeT")
        AT = cxD.sb([128, 8, CAP], BF16, "AT")
        Yb = cxD.sb([128, 2, D], BF16, "Yb")
        wsl = [cxD.sb([128, 2], F32, "wsl%d" % i) for i in range(2)]
        sgl = [cxD.sb([128, CAP], F32, "sgl%d" % i) for i in range(2)]
        wgp = [cxD.sb([128, 16, 256], BF16, "wgp%d" % i) for i in range(2)]
        wup = [cxD.sb([128, 16, 256], BF16, "wup%d" % i) for i in range(2)]
        wdp = [cxD.sb([128, 8, 512], BF16, "wdp%d" % i) for i in range(2)]
        allX = [XK(b) for b in range(TB)]
        pcnt = [0]
        dcnt = [0]

        def expert(ex):
            s = ex % 2
            Sk = lambda b: ("S", s, b)
            for b in range(TB):
                P.dve(lambda e, b=b: e.tensor_scalar(out=S[s][:, b, :], in0=iota_f[:], scalar1=POS[:, b, ex:ex + 1],
                                                     scalar2=MSK[:, b, ex:ex + 1], op0=ALU.is_equal, op1=ALU.mult),
                      reads=["pos", "msk", "iota_f"], writes=[Sk(b)])
            bw = nbank()
            for jb in range(2):
                for b in range(TB):
                    P.pe(lambda e, jb=jb, b=b: e.matmul(fb[bw][:, 2 * jb:2 * jb + 2], S[s][:, b, jb * 128:(jb + 1) * 128],
                                                        WHL[:, b, ex, :], start=(jb == 0 and b == 0), stop=(b == TB - 1),
                                                        skip_group_check=True),
                         reads=[Sk(b), "whl0", "whl1"], writes=[fk[bw]])
            P.dve(lambda e: e.tensor_reduce(out=wsl[s][:], in_=fb[bw][:, 0:4].rearrange("p (j k) -> p j k", k=2),
                                            axis=AX.X, op=ALU.add), reads=[fk[bw]], writes=[("wsl", s)])
            for jb in range(2):
                for b in range(TB):
                    t = tb[0] if b < 8 else tb[1]
                    tkk = tk[0] if b < 8 else tk[1]
                    off = (b % 8) * 128
                    P.pe(lambda e, jb=jb, b=b, t=t, off=off: e.transpose(t[:, off:off + 128], S[s][:, b, jb * 128:(jb + 1) * 128],
                                                                        C["ident_b"][:]),
                         reads=[Sk(b), K["ident_b"]], writes=[tkk])
                P.dve(lambda e, jb=jb: e.tensor_copy(out=ST[s][:, jb, 0:1024], in_=tb[0][:, :]), reads=[tk[0]], writes=[("ST", s, jb, 0)])
                P.act(lambda e, jb=jb: e.activation(out=ST[s][:, jb, 1024:NT], in_=tb[1][:, 0:128], func=AF.Copy),
                      reads=[tk[1]], writes=[("ST", s, jb, 1)])
            for c in range(16):
                bgx = nbank()
                for b in range(TB):
                    P.pe(lambda e, c=c, b=b, bgx=bgx: e.matmul(fb[bgx][:, 0:CAP], Hb[:, b, c * 128:(c + 1) * 128], S[s][:, b, :],
                                                               start=(b == 0), stop=(b == TB - 1)),
                         reads=[("Hb", b), Sk(b)], writes=[fk[bgx]])
                if c % 2 == 0:
                    P.dve(lambda e, c=c, bgx=bgx: e.tensor_copy(out=XeT[:, c, :], in_=fb[bgx][:, 0:CAP]),
                          reads=[fk[bgx]], writes=[("XeT", c)])
                else:
                    P.act(lambda e, c=c, bgx=bgx: e.activation(out=XeT[:, c, :], in_=fb[bgx][:, 0:CAP], func=AF.Copy),
                          reads=[fk[bgx]], writes=[("XeT", c)])
            gv = io["wgate"][ex].rearrange("(c p) f -> p c f", p=128)
            uv = io["wup"][ex].rearrange("(c p) f -> p c f", p=128)
            for pc in range(4):
                w = pcnt[0] % 2
                pcnt[0] += 1
                for hf in range(2):
                    P.dma(lambda e, w=w, pc=pc, hf=hf: e.dma_start(out=wgp[w][:, 8 * hf:8 * hf + 8, :],
                                                                  in_=gv[:, 8 * hf:8 * hf + 8, pc * 256:(pc + 1) * 256]),
                          writes=[("wgp", w, hf)], q="pool")
                    P.dma(lambda e, w=w, pc=pc, hf=hf: e.dma_start(out=wup[w][:, 8 * hf:8 * hf + 8, :],
                                                                  in_=uv[:, 8 * hf:8 * hf + 8, pc * 256:(pc + 1) * 256]),
                          writes=[("wup", w, hf)], q="pool")
                for fl in range(2):
                    fc = pc * 2 + fl
                    bg, bu = nbank(), nbank()
                    for c in range(16):
                        P.pe(lambda e, c=c, w=w, fl=fl, bg=bg: e.matmul(fb[bg][:, 0:CAP], wgp[w][:, c, fl * 128:(fl + 1) * 128],
                                                                        XeT[:, c, :], start=(c == 0), stop=(c == 15)),
                             reads=[("wgp", w, c // 8), ("XeT", c)], writes=[fk[bg]])
                    for c in range(16):
                        P.pe(lambda e, c=c, w=w, fl=fl, bu=bu: e.matmul(fb[bu][:, 0:CAP], wup[w][:, c, fl * 128:(fl + 1) * 128],
                                                                        XeT[:, c, :], start=(c == 0), stop=(c == 15)),
                             reads=[("wup", w, c // 8), ("XeT", c)], writes=[fk[bu]])
                    u = fc % 2
                    P.act(lambda e, u=u, bg=bg: e.activation(out=sgl[u][:], in_=fb[bg][:, 0:CAP], func=AF.Silu),
                          reads=[fk[bg]], writes=[("sgl", u)])
                    P.dve(lambda e, u=u, bu=bu, fc=fc: e.tensor_mul(out=AT[:, fc, :], in0=sgl[u][:], in1=fb[bu][:, 0:CAP]),
                          reads=[("sgl", u), fk[bu]], writes=[("AT", fc)])
            dv = io["wdown"][ex].rearrange("(c p) n -> p c n", p=128)
            ATk = [("AT", fc) for fc in range(8)]
            for cg in range(4):
                w = dcnt[0] % 2
                dcnt[0] += 1
                for hf in range(2):
                    P.dma(lambda e, w=w, cg=cg, hf=hf: e.dma_start(out=wdp[w][:, 4 * hf:4 * hf + 4, :],
                                                                  in_=dv[:, 4 * hf:4 * hf + 4, cg * 512:(cg + 1) * 512]),
                          writes=[("wdp", w, hf)], q="pool")
                for jb in range(2):
                    by = nbank()
                    for fc in range(8):
                        P.pe(lambda e, fc=fc, jb=jb, w=w, by=by: e.matmul(fb[by][:, :], AT[:, fc, jb * 128:(jb + 1) * 128],
                                                                          wdp[w][:, fc, :], start=(fc == 0), stop=(fc == 7)),
                             reads=[("AT", fc), ("wdp", w, fc // 4)], writes=[fk[by]])
                    if jb == 0:
                        P.act(lambda e, jb=jb, cg=cg, by=by: e.activation(out=Yb[:, jb, cg * 512:(cg + 1) * 512], in_=fb[by][:, :],
                                                                          func=AF.Copy, scale=wsl[s][:, jb:jb + 1]),
                              reads=[fk[by], ("wsl", s)], writes=[("Yb", jb, cg)])
                    else:
                        P.dve(lambda e, jb=jb, cg=cg, by=by: e.tensor_scalar(out=Yb[:, jb, cg * 512:(cg + 1) * 512], in0=fb[by][:, :],
                                                                             scalar1=wsl[s][:, jb:jb + 1], scalar2=None, op0=ALU.mult),
                              reads=[fk[by], ("wsl", s)], writes=[("Yb", jb, cg)])
            for cg in range(4):
                for b in range(TB):
                    bc_ = nbank()
                    for jb in range(2):
                        P.pe(lambda e, jb=jb, b=b, cg=cg, bc_=bc_: e.matmul(fb[bc_][:, :], ST[s][:, jb, b * 128:(b + 1) * 128],
                                                                            Yb[:, jb, cg * 512:(cg + 1) * 512],
                                                                            start=(jb == 0), stop=(jb == 1)),
                             reads=[("ST", s, jb, 0 if b < 8 else 1), ("Yb", jb, cg)], writes=[fk[bc_]])
                    P.dve(lambda e, b=b, cg=cg, bc_=bc_: e.tensor_add(out=X[:, b, cg * 512:(cg + 1) * 512],
                                                                      in0=X[:, b, cg * 512:(cg + 1) * 512], in1=fb[bc_][:, :]),
                          reads=[XK(b), fk[bc_]], writes=[XK(b)])

        import os
        for ex in range(int(os.environ.get("T_NEXP", NE))):
            expert(ex)
        P.barrier()
        stD.close()

        layer_norm(io["lnf"], cx)
        write_outputs(Hb, cx)
        P.emit()


_PROGS = {}


def _prog(name):
    if name not in _PROGS:
        _PROGS[name] = {"T0": lambda: build_T(True), "A": build_A, "T": lambda: build_T(False)}[name]()
    return _PROGS[name]


def _run(name, in_maps):
    res = run_bass_kernel_spmd(_prog(name), in_maps, core_ids=list(range(NCORE)))
    return res.results


def _gather_hT(outs):
    full = np.empty((D, L), dtype=ml_dtypes.bfloat16)
    full[:, 0:128] = np.asarray(outs[0]["hT"])[:, 0:128]
    for c in range(NCORE):
        full[:, 128 + 1024 * c:128 + 1024 * (c + 1)] = np.asarray(outs[c]["hT"])[:, 128:NT]
    return full


def kernel(x, meta_tokens, ln_in_g, ln_in_b, w_in, b_forget, w_branch_sb, w_branch_fox, w_out,
           ln_mix_g, ln_mix_b, w_router, router_bias, w_gate, w_up, w_down, ln_ffn_g, ln_ffn_b):
    f32 = np.float32
    x = np.asarray(x, f32)
    w_in = np.asarray(w_in, f32)
    depth = w_in.shape[0]
    blk0 = np.concatenate([np.zeros((NPAD, D), f32), np.asarray(meta_tokens, f32)], 0)
    lng = np.stack([np.asarray(ln_in_g, f32), np.asarray(ln_in_b, f32)])
    t_in = [{"xin": np.ascontiguousarray(np.concatenate([blk0, x[0, 1024 * c:1024 * (c + 1)]], 0)), "lng": lng}
            for c in range(NCORE)]
    outs = _run("T0", t_in)
    cols = [np.concatenate([np.arange(0, 128), 128 + 1024 * c + np.arange(1024)]) for c in range(NCORE)]
    for i in range(depth):
        hT_all = _gather_hT(outs)
        a_in = []
        for c in range(NCORE):
            s = slice(c * 128, (c + 1) * 128)
            wA = np.concatenate([w_in[i][:, 0:1024][:, s], w_in[i][:, 1024:2048][:, s],
                                 w_in[i][:, 3072:4096][:, s], w_in[i][:, 4096:5120][:, s],
                                 w_in[i][:, 2048:3072][:, s], w_in[i][:, 5120:6144][:, s],
                                 w_in[i][:, 6144 + c:6145 + c]], 1)
            a_in.append({"hT": hT_all, "wA": np.ascontiguousarray(wA),
                         "bf": np.asarray(b_forget, f32)[i, c].reshape(1, 1)})
        a_out = _run("A", a_in)
        del a_in, hT_all
        Y = np.empty((D, L), dtype=ml_dtypes.bfloat16)
        for c in range(NCORE):
            y = np.asarray(a_out[c]["yT"])
            Y[c * 128:(c + 1) * 128] = y[0:128]
            Y[1024 + c * 128:1024 + (c + 1) * 128] = y[128:256]
        wg = np.ascontiguousarray(w_in[i][:, 6152:6152 + 2 * D])
        shared = {"wg": wg, "wbs": np.asarray(w_branch_sb, f32)[i], "wbf": np.asarray(w_branch_fox, f32)[i],
                  "wo": np.asarray(w_out, f32)[i],
                  "lnm": np.stack([np.asarray(ln_mix_g, f32)[i], np.asarray(ln_mix_b, f32)[i]]),
                  "wr": np.asarray(w_router, f32), "rb": np.asarray(router_bias, f32).reshape(1, NE),
                  "wgate": np.asarray(w_gate, f32)[i], "wup": np.asarray(w_up, f32)[i],
                  "wdown": np.asarray(w_down, f32)[i],
                  "lnf": np.stack([np.asarray(ln_ffn_g, f32)[i], np.asarray(ln_ffn_b, f32)[i]])}
        t_in = []
        for c in range(NCORE):
            m = dict(shared)
            m["hprev"] = np.asarray(outs[c]["h"])
            m["hTp"] = np.asarray(outs[c]["hT"])
            m["yT"] = np.ascontiguousarray(Y[:, cols[c]])
            t_in.append(m)
        outs = _run("T", t_in)
        del t_in, Y
    out = np.empty((1, SEQ, D), f32)
    for c in range(NCORE):
        out[0, 1024 * c:1024 * (c + 1)] = np.asarray(outs[c]["h"])[128:NT]
    return out
```

```python
import numpy as np
import ml_dtypes
import concourse.bass as bass
import concourse.mybir as mybir
from concourse.bass_utils import run_bass_kernel_spmd

F32 = mybir.dt.float32
BF16 = mybir.dt.bfloat16
I32 = mybir.dt.int32
AF = mybir.ActivationFunctionType
ALU = mybir.AluOpType
AX = mybir.AxisListType

D = 2048
SEQ = 8192
L = SEQ + 128
NBLK = L // 128
DH = 128
NPAD = 112
NE = 16
FF = 1024
CAP = 256
ALPHA = 4.0 ** 0.25
EPS = 1e-5
NCORE = 8
TB = 9
NT = TB * 128


class Prog:
    SELFSYNC = ("act", "dve", "pool")

    def __init__(self, nc, n_dma_sems=12):
        self.nc = nc
        self.ops = []
        self.res_w = {}
        self.res_r = {}
        self.n_dma_sems = n_dma_sems

    def add(self, eng, fn, reads=(), writes=(), dma=False):
        idx = len(self.ops)
        deps = set()
        ex = [r for r in reads if isinstance(r, str) and r.startswith("bank")]
        if ex:
            reads = [r for r in reads if r not in ex]
            writes = list(writes) + [r for r in ex if r not in writes]
        for r in reads:
            w = self.res_w.get(r)
            if w is not None:
                deps.add(w)
        for w_ in writes:
            w = self.res_w.get(w_)
            if w is not None:
                deps.add(w)
            for rd in self.res_r.get(w_, ()):
                deps.add(rd)
        deps.discard(idx)
        self.ops.append(dict(eng=eng, fn=fn, deps=deps, dma=dma, sig=dma))
        for r in reads:
            self.res_r.setdefault(r, []).append(idx)
        for w_ in writes:
            self.res_w[w_] = idx
            self.res_r[w_] = []
        return idx

    def pe(self, fn, reads=(), writes=()):
        return self.add("pe", fn, reads, writes)

    def act(self, fn, reads=(), writes=()):
        return self.add("act", fn, reads, writes)

    def dve(self, fn, reads=(), writes=()):
        return self.add("dve", fn, reads, writes)

    def pool(self, fn, reads=(), writes=()):
        return self.add("pool", fn, reads, writes)

    def dma(self, fn, reads=(), writes=(), q="sp"):
        return self.add(q, fn, reads, writes, dma=True)

    def barrier(self):
        last = {}
        dmas = set()
        for i, x in enumerate(self.ops):
            if x["fn"] is None:
                continue
            if x["dma"]:
                dmas.add(i)
            else:
                last[x["eng"]] = i
        deps = set(last.values()) | dmas
        for e in ("pe", "act", "dve", "pool", "sp"):
            self.ops.append(dict(eng=e, fn=None, deps=set(deps), dma=False, sig=False, bar=True))

    def emit(self, final_waits=True):
        nc = self.nc
        ops = self.ops
        for x in ops:
            for d in x["deps"]:
                dd = ops[d]
                if dd["dma"]:
                    continue
                if dd["eng"] != x["eng"] or x["dma"] or dd["eng"] in self.SELFSYNC or x.get("bar"):
                    dd["sig"] = True
        engs = ["pe", "act", "dve", "pool", "sp"]
        import contextlib
        with contextlib.ExitStack() as st:
            esem = {e: st.enter_context(nc.semaphore("sem_" + e)) for e in engs}
            dsem = {q: [st.enter_context(nc.semaphore("dsem_%s_%d" % (q, j)))
                        for j in range(self.n_dma_sems)] for q in ("sp", "pool", "act")}
            ecount = {e: 0 for e in engs}
            dnext = {q: 0 for q in dsem}
            duse = {q: [0] * self.n_dma_sems for q in dsem}
            for x in ops:
                if x["dma"]:
                    q = x["eng"]
                    j = dnext[q]
                    dnext[q] = (j + 1) % self.n_dma_sems
                    duse[q][j] += 1
                    x["sem"] = dsem[q][j]
                    x["val"] = 16 * duse[q][j]
                    x["prev"] = 16 * (duse[q][j] - 1)
                elif x["sig"]:
                    ecount[x["eng"]] += 1
                    x["sem"] = esem[x["eng"]]
                    x["val"] = ecount[x["eng"]]
            block = st.enter_context(nc.Block())
            per_eng = {e: [x for x in ops if x["eng"] == e] for e in engs}
            last_dma = [x for x in ops if x["dma"]]

            def run(e, eng):
                waited = {}

                def wait(sem, val):
                    key = id(sem)
                    if waited.get(key, 0) >= val:
                        return
                    waited[key] = val
                    eng.wait_ge(sem, val)

                for x in per_eng[e]:
                    for d in sorted(x["deps"]):
                        dd = ops[d]
                        if (not dd["dma"]) and dd["eng"] == e and not x["dma"] and e not in self.SELFSYNC \
                                and not x.get("bar"):
                            continue
                        wait(dd["sem"], dd["val"])
                    if x["fn"] is None:
                        continue
                    if x["dma"] and x["prev"] > 0:
                        wait(x["sem"], x["prev"])
                    ins = x["fn"](eng)
                    if x["dma"]:
                        ins.then_inc(x["sem"], 16)
                    elif x["sig"]:
                        ins.then_inc(x["sem"], 1)
                if e == "sp" and final_waits:
                    for q in dsem:
                        for j in range(self.n_dma_sems):
                            if duse[q][j] > 0:
                                eng.wait_ge(dsem[q][j], 16 * duse[q][j])

            @block.tensor
            def _(eng):
                run("pe", eng)

            @block.scalar
            def _(eng):
                run("act", eng)

            @block.vector
            def _(eng):
                run("dve", eng)

            @block.gpsimd
            def _(eng):
                run("pool", eng)

            @block.sync
            def _(eng):
                run("sp", eng)


class Ctx:
    def __init__(self, nc, stack):
        self.nc = nc
        self.stack = stack
        self.n = 0

    def sb(self, shape, dt, name=None):
        self.n += 1
        return self.stack.enter_context(self.nc.sbuf_tensor(name or ("t%d" % self.n), list(shape), dt))

    def ps(self, shape, dt=F32, name=None):
        self.n += 1
        return self.stack.enter_context(self.nc.psum_tensor(name or ("p%d" % self.n), list(shape), dt))


def make_consts(P, cx, names):
    c = {}

    def tri(name, cmp_op, dt, val, flip=False, n=128):
        sgn = -1 if flip else 1
        tf = cx.sb([128, n], F32, name + "_f")
        P.pool(lambda e: e.memset(tf[:], val), writes=[name + "_f"])
        P.pool(lambda e: e.affine_select(out=tf[:], in_=tf[:], pattern=[[sgn, n]], compare_op=cmp_op,
                                         fill=0.0, base=0, channel_multiplier=-sgn),
               reads=[name + "_f"], writes=[name + "_f"])
        if dt == F32:
            c[name] = tf
            return name + "_f"
        tb = cx.sb([128, n], dt, name)
        P.pool(lambda e: e.tensor_copy(out=tb[:], in_=tf[:]), reads=[name + "_f"], writes=[name])
        c[name] = tb
        return name

    spec = {
        "negge": (ALU.is_ge, BF16, -1.0, True),
        "neglt": (ALU.is_gt, BF16, -1.0),
        "triincl": (ALU.is_ge, F32, 1.0),
        "ustrict": (ALU.is_gt, F32, 1.0),
        "ustrict_b": (ALU.is_gt, BF16, 1.0),
        "mstrict": (ALU.is_gt, F32, 1.0),
        "mincl": (ALU.is_ge, BF16, 1.0),
        "ident_f": (ALU.is_equal, F32, 1.0),
        "ident_b": (ALU.is_equal, BF16, 1.0),
    }
    c["_key"] = {}
    for nm in names:
        if nm in spec:
            c["_key"][nm] = tri(nm, *spec[nm])
        elif nm == "ones_b":
            t = cx.sb([128, 128], BF16, "ones_b")
            P.pool(lambda e, t=t: e.memset(t[:], 1.0), writes=["ones_b"])
            c[nm] = t
            c["_key"][nm] = "ones_b"
        elif nm == "ones_f":
            t = cx.sb([128, 128], F32, "ones_f")
            P.pool(lambda e, t=t: e.memset(t[:], 1.0), writes=["ones_f"])
            c[nm] = t
            c["_key"][nm] = "ones_f"
    return c


def build_A(upto=3):
    import contextlib
    nc = bass.Bass("TRN2", target_bir_lowering=False)
    hT = nc.dram_tensor("hT", [D, L], BF16, kind="ExternalInput").ap()
    wA = nc.dram_tensor("wA", [D, 769], F32, kind="ExternalInput").ap()
    bfg = nc.dram_tensor("bf", [1, 1], F32, kind="ExternalInput").ap()
    yT = nc.dram_tensor("yT", [256, L], BF16, kind="ExternalOutput").ap()
    cscr = nc.dram_tensor("cscr", [6, L], BF16, kind="Internal").ap()
    emit_A(nc, hT, wA, bfg, yT, cscr, upto)
    return nc


def emit_A(nc, hT, wA, bfg, yT, cscr, upto=3):
    import contextlib
    with contextlib.ExitStack() as st:
        cx = Ctx(nc, st)
        P = Prog(nc)
        C = make_consts(P, cx, ["negge", "neglt", "triincl", "ustrict", "mstrict", "mincl",
                                "ident_f", "ones_b", "ones_f"])
        K = C["_key"]
        scale = DH ** -0.5

        QT = [cx.sb([128, L], BF16, "QT%d" % i) for i in range(2)]
        KT = [cx.sb([128, L], BF16, "KT%d" % i) for i in range(2)]
        V = [cx.sb([128, NBLK, 128], BF16, "V%d" % i) for i in range(2)]
        FL = cx.sb([128, NBLK], F32, "FL")
        negb = cx.sb([128, 1], F32, "negb")
        banks = [cx.ps([128, 512], F32, "bank%d" % i) for i in range(8)]
        bk = ["bank%d" % i for i in range(8)]
        st1 = contextlib.ExitStack()
        cx1 = Ctx(nc, st1)
        W = cx1.sb([128, 16, 772], BF16, "W")
        hTt = [cx1.sb([128, 16, 512], BF16, "hTt%d" % i) for i in range(2)]

        wv = wA.rearrange("(c p) n -> p c n", p=128)
        for c4 in range(4):
            P.dma(lambda e, c4=c4: e.dma_start(out=W[:, 4 * c4:4 * c4 + 4, 0:769], in_=wv[:, 4 * c4:4 * c4 + 4, :]),
                  writes=[("W", c4)], q="pool")
        P.dma(lambda e: e.dma_start(out=negb[:], in_=bfg.partition_broadcast(128)), writes=["negb"], q="sp")
        P.dve(lambda e: e.tensor_scalar(out=negb[:], in0=negb[:], scalar1=-1.0, scalar2=None, op0=ALU.mult),
              reads=["negb"], writes=["negb"])
        Wk = [("W", i) for i in range(4)]

        tiles = [(0, 1)] + [(1 + 4 * i, 4) for i in range(16)]
        hv = hT.rearrange("(c p) t -> p c t", p=128)

        rr = 0
        import os
        _nt = int(os.environ.get("A_NT", "17"))
        _dov = int(os.environ.get("A_DOV", "1"))
        _doq = int(os.environ.get("A_DOQ", "1"))
        for ti, (qb0, nb) in enumerate(tiles):
            if ti >= _nt:
                break
            t0 = qb0 * 128
            n = nb * 128
            hb = hTt[ti % 2]
            hk = "hTt%d" % (ti % 2)
            for c4 in range(4):
                P.dma(lambda e, hb=hb, t0=t0, n=n, c4=c4: e.dma_start(
                    out=hb[:, 4 * c4:4 * c4 + 4, 0:n], in_=hv[:, 4 * c4:4 * c4 + 4, t0:t0 + n]),
                    writes=[(hk, c4)], q="sp")
            for which, (dst, col, sc) in enumerate([(QT[0], 0, scale), (KT[0], 128, 1.0),
                                                    (QT[1], 256, scale), (KT[1], 384, 1.0)][:4 * _doq]):
                b = rr % 8
                rr += 1
                for c in range(16):
                    P.pe(lambda e, b=b, c=c, col=col, hb=hb, n=n: e.matmul(
                        banks[b][:, 0:n], W[:, c, col:col + 128], hb[:, c, 0:n],
                        start=(c == 0), stop=(c == 15)),
                        reads=Wk + [(hk, c // 4)], writes=[bk[b]])
                dk = ("QK", which, ti)
                if which % 2 == 0:
                    P.act(lambda e, b=b, dst=dst, t0=t0, n=n, sc=sc: e.activation(
                        out=dst[:, t0:t0 + n], in_=banks[b][:, 0:n], func=AF.Copy, scale=sc),
                        reads=[bk[b]], writes=[dk])
                else:
                    P.dve(lambda e, b=b, dst=dst, t0=t0, n=n: e.tensor_copy(out=dst[:, t0:t0 + n], in_=banks[b][:, 0:n]),
                          reads=[bk[b]], writes=[dk])
            for j in range(nb * _dov):
                blk = qb0 + j
                b = rr % 8
                rr += 1
                for c in range(16):
                    P.pe(lambda e, b=b, c=c, hb=hb, j=j: e.matmul(
                        banks[b][:, 0:257], hb[:, c, j * 128:(j + 1) * 128], W[:, c, 512:769],
                        start=(c == 0), stop=(c == 15)),
                        reads=Wk + [(hk, c // 4)], writes=[bk[b]])
                P.act(lambda e, b=b, blk=blk: e.activation(out=V[0][:, blk, :], in_=banks[b][:, 0:128], func=AF.Copy),
                      reads=[bk[b]], writes=[("V", 0, blk)])
                P.dve(lambda e, b=b, blk=blk: e.tensor_copy(out=V[1][:, blk, :], in_=banks[b][:, 128:256]),
                      reads=[bk[b]], writes=[("V", 1, blk)])
                P.dve(lambda e, b=b, blk=blk: e.tensor_copy(out=FL[:, blk:blk + 1], in_=banks[b][:, 256:257]),
                      reads=[bk[b]], writes=["FL"])

        if upto < 2:
            P.emit()
            st1.close()
            return nc
        P.barrier()
        st1.close()
        spf = cx.sb([128, NBLK], F32, "spf")
        P.act(lambda e: e.activation(out=spf[:], in_=FL[:], func=AF.Exp, bias=negb[:, 0:1], scale=-1.0),
              reads=["FL", "negb"], writes=["spf"])
        P.act(lambda e: e.activation(out=spf[:], in_=spf[:], func=AF.Ln, bias=1.0, scale=1.0),
              reads=["spf"], writes=["spf"])
        P.dve(lambda e: e.memset(spf[0:NPAD, 0:1], 0.0), reads=["spf"], writes=["spf"])
        P.pe(lambda e: e.matmul(banks[0][0:NBLK, 0:128], spf[:, :], C["ones_f"][:, :], start=True, stop=True),
             reads=["spf", K["ones_f"]], writes=[bk[0]])
        abc = cx.sb([NBLK, 128], F32, "abc")
        P.dve(lambda e: e.tensor_copy(out=abc[:], in_=banks[0][0:NBLK, 0:128]), reads=[bk[0]], writes=["abc"])
        P.pe(lambda e: e.matmul(banks[1][0:NBLK, 0:128], C["ustrict"][0:NBLK, 0:NBLK], abc[:, :],
                                start=True, stop=False), reads=["abc", K["ustrict"]], writes=[bk[1]])
        P.pe(lambda e: e.matmul(banks[1][0:NBLK, 0:128], spf[:, :], C["triincl"][:, :],
                                start=False, stop=True), reads=["spf", K["triincl"]], writes=[bk[1]])
        cp = cx.sb([NBLK, 128], F32, "cp")
        P.dve(lambda e: e.tensor_copy(out=cp[:], in_=banks[1][0:NBLK, 0:128]), reads=[bk[1]], writes=["cp"])
        parts_p = cx.sb([NBLK, 3, 128], BF16, "parts_p")
        parts_n = cx.sb([NBLK, 3, 128], BF16, "parts_n")
        tmpf = cx.sb([NBLK, 128], F32, "tmpf")
        for k in range(3):
            P.dve(lambda e, k=k: e.tensor_copy(out=parts_p[:, k, :], in_=cp[:]), reads=["cp"], writes=["parts_p"])
            P.dve(lambda e, k=k: e.tensor_copy(out=tmpf[:], in_=parts_p[:, k, :]), reads=["parts_p"], writes=["tmpf"])
            P.dve(lambda e, k=k: e.tensor_sub(out=cp[:], in0=cp[:], in1=tmpf[:]), reads=["cp", "tmpf"], writes=["cp"])
        P.dve(lambda e: e.tensor_scalar(out=parts_n[:], in0=parts_p[:], scalar1=-1.0, scalar2=None, op0=ALU.mult),
              reads=["parts_p"], writes=["parts_n"])
        P.dve(lambda e: e.memset(parts_p[0:1, 0, 0:NPAD], -30000.0), reads=["parts_p", "parts_n"], writes=["parts_p"])
        LK = cx.sb([6, L], BF16, "LK")
        RQ = cx.sb([6, L], BF16, "RQ")
        P.pool(lambda e: e.memset(LK[:], 1.0), writes=["LK"])
        P.pool(lambda e: e.memset(RQ[:], 1.0), writes=["RQ"])
        P.dma(lambda e: e.dma_start(out=cscr[0:3, :].rearrange("k (b p) -> b k p", p=128), in_=parts_p[:]),
              reads=["parts_p"], writes=["cscr_p"], q="sp")
        P.dma(lambda e: e.dma_start(out=cscr[3:6, :].rearrange("k (b p) -> b k p", p=128), in_=parts_n[:]),
              reads=["parts_n"], writes=["cscr_n"], q="sp")
        P.dma(lambda e: e.dma_start(out=LK[3:6, :], in_=cscr[0:3, :]), reads=["cscr_p", "LK"], writes=["LK"], q="sp")
        P.dma(lambda e: e.dma_start(out=RQ[0:3, :], in_=cscr[3:6, :]), reads=["cscr_n", "RQ"], writes=["RQ"], q="sp")

        if upto < 3:
            P.emit()
            return nc
        e_t = [cx.sb([128, 512], F32, "e%d" % i) for i in range(2)]
        sp_t = [cx.sb([128, 512], BF16, "sp%d" % i) for i in range(3)]
        g_t = [cx.sb([128, 512], F32, "g%d" % i) for i in range(2)]
        w_t = [cx.sb([128, 512], BF16, "w%d" % i) for i in range(2)]
        p_t = [cx.sb([128, 512], BF16, "pp%d" % i) for i in range(2)]
        yst = [cx.sb([128, 512], BF16, "yst%d" % i) for i in range(4)]
        rec = cx.sb([128, 512], F32, "rec")
        ZB = [0, 1]
        AB, OB = 2, 3
        SB_ = [4, 5]
        O2, DB = 6, 7

        def do_tile(ti, qb0, nb):
            t0 = qb0 * 128
            n = nb * 128
            nsteps = qb0 + nb

            def kkeys(jb):
                tj = 0 if jb == 0 else 1 + (jb - 1) // 4
                return tj

            P.dve(lambda e, n=n: e.memset(banks[AB][:, 0:n], 0.0), writes=[bk[AB]])
            P.dve(lambda e, n=n: e.memset(banks[OB][:, 0:n], 0.0), writes=[bk[OB]])
            P.dve(lambda e, n=n: e.memset(banks[O2][:, 0:n], 0.0), writes=[bk[O2]])
            P.dve(lambda e, n=n: e.memset(banks[DB][:, 0:n], 0.0), writes=[bk[DB]])

            def cols(i):
                jb = qb0 + nb - 1 - i
                r = jb - qb0
                c0 = max(r, 0) * 128
                return jb, r, c0

            def sbA(i):
                jb, r, c0 = cols(i)
                zb = ZB[i % 2]
                eb = e_t[i % 2]
                ek = "e%d" % (i % 2)
                spb = sp_t[i % 3]
                spk = "sp%d" % (i % 3)
                tj = kkeys(jb)
                P.pe(lambda e: e.matmul(banks[zb][:, c0:n], KT[0][:, jb * 128:(jb + 1) * 128], QT[0][:, t0 + c0:t0 + n],
                                        start=True, stop=True),
                     reads=[("QK", 1, tj), ("QK", 0, ti)], writes=[bk[zb]])
                P.act(lambda e: e.activation(out=eb[:, c0:n], in_=banks[zb][:, c0:n], func=AF.Exp),
                      reads=[bk[zb]], writes=[ek])
                if r >= 0:
                    P.dve(lambda e: e.tensor_mul(out=eb[:, c0:c0 + 128], in0=eb[:, c0:c0 + 128], in1=C["mstrict"][:]),
                          reads=[ek, K["mstrict"]], writes=[ek])
                if jb == 0:
                    P.dve(lambda e: e.memset(eb[0:NPAD, c0:n], 0.0), reads=[ek], writes=[ek])
                P.act(lambda e: e.activation(out=spb[:, c0:n], in_=eb[:, c0:n], func=AF.Ln, bias=1.0, scale=1.0),
                      reads=[ek], writes=[spk])

            def sbB(i):
                jb, r, c0 = cols(i)
                spb = sp_t[i % 3]
                spk = "sp%d" % (i % 3)
                eb = e_t[i % 2]
                ek = "e%d" % (i % 2)
                gb = g_t[i % 2]
                gk = "g%d" % (i % 2)
                wb = w_t[i % 2]
                wk = "w%d" % (i % 2)
                if i > 0:
                    _, _, pc0 = cols(i - 1)
                    spp = sp_t[(i - 1) % 3]
                    sppk = "sp%d" % ((i - 1) % 3)
                    P.pe(lambda e: e.matmul(banks[AB][:, pc0:n], C["neglt"][:], spp[:, pc0:n],
                                            start=False, stop=True, skip_group_check=True),
                         reads=[sppk, K["neglt"]], writes=[bk[AB]])
                P.pe(lambda e: e.matmul(banks[AB][:, c0:n], C["negge"][:], spb[:, c0:n],
                                        start=False, stop=True, skip_group_check=True),
                     reads=[spk, K["negge"]], writes=[bk[AB]])
                P.act(lambda e: e.activation(out=gb[:, c0:n], in_=banks[AB][:, c0:n], func=AF.Exp),
                      reads=[bk[AB]], writes=[gk])
                P.dve(lambda e: e.tensor_mul(out=wb[:, c0:n], in0=eb[:, c0:n], in1=gb[:, c0:n]),
                      reads=[ek, gk], writes=[wk])

            def sbC(i):
                jb, r, c0 = cols(i)
                wb = w_t[i % 2]
                wk = "w%d" % (i % 2)
                P.pe(lambda e: e.matmul(banks[OB][:, c0:n], V[0][:, jb, :], wb[:, c0:n],
                                        start=False, stop=True, skip_group_check=True),
                     reads=[wk, ("V", 0, jb)], writes=[bk[OB]])

            def fxA(i):
                jb, r, c0 = cols(i)
                sb = SB_[i % 2]
                pb = p_t[i % 2]
                pk = "pp%d" % (i % 2)
                tj = kkeys(jb)
                P.pe(lambda e: e.matmul(banks[sb][:, c0:n], KT[1][:, jb * 128:(jb + 1) * 128], QT[1][:, t0 + c0:t0 + n],
                                        start=True, stop=False),
                     reads=[("QK", 3, tj), ("QK", 2, ti)], writes=[bk[sb]])
                P.pe(lambda e: e.matmul(banks[sb][:, c0:n], LK[0:6, jb * 128:(jb + 1) * 128], RQ[0:6, t0 + c0:t0 + n],
                                        start=False, stop=True),
                     reads=["LK", "RQ"], writes=[bk[sb]])
                P.act(lambda e: e.activation(out=pb[:, c0:n], in_=banks[sb][:, c0:n], func=AF.Exp),
                      reads=[bk[sb]], writes=[pk])
                if r >= 0:
                    P.dve(lambda e: e.tensor_mul(out=pb[:, c0:c0 + 128], in0=pb[:, c0:c0 + 128], in1=C["mincl"][:]),
                          reads=[pk, K["mincl"]], writes=[pk])

            def fxB(i):
                jb, r, c0 = cols(i)
                pb = p_t[i % 2]
                pk = "pp%d" % (i % 2)
                P.pe(lambda e: e.matmul(banks[O2][:, c0:n], V[1][:, jb, :], pb[:, c0:n],
                                        start=False, stop=True, skip_group_check=True),
                     reads=[pk, ("V", 1, jb)], writes=[bk[O2]])
                P.pe(lambda e: e.matmul(banks[DB][:, c0:n], C["ones_b"][:], pb[:, c0:n],
                                        start=False, stop=True, skip_group_check=True),
                     reads=[pk, K["ones_b"]], writes=[bk[DB]])

            for it in range(nsteps + 2):
                if it < nsteps:
                    sbA(it)
                    fxA(it)
                if 0 <= it - 1 < nsteps:
                    sbB(it - 1)
                    fxB(it - 1)
                if 0 <= it - 2 < nsteps:
                    sbC(it - 2)

            ys = yst[(2 * ti) % 4]
            ysk = "yst%d" % ((2 * ti) % 4)
            yf = yst[(2 * ti + 1) % 4]
            yfk = "yst%d" % ((2 * ti + 1) % 4)
            P.act(lambda e, ys=ys, n=n: e.activation(out=ys[:, 0:n], in_=banks[OB][:, 0:n], func=AF.Copy),
                  reads=[bk[OB]], writes=[ysk])
            P.dma(lambda e, ys=ys, n=n, t0=t0: e.dma_start(out=yT[0:128, t0:t0 + n], in_=ys[:, 0:n]),
                  reads=[ysk], writes=[("yT", 0, ti)], q="sp")
            P.dve(lambda e, n=n: e.tensor_scalar(out=rec[:, 0:n], in0=banks[DB][:, 0:n], scalar1=1e-30, scalar2=None,
                                                 op0=ALU.add), reads=[bk[DB]], writes=["rec"])
            P.dve(lambda e, n=n: e.reciprocal(out=rec[:, 0:n], in_=rec[:, 0:n]), reads=["rec"], writes=["rec"])
            P.dve(lambda e, yf=yf, n=n: e.tensor_mul(out=yf[:, 0:n], in0=banks[O2][:, 0:n], in1=rec[:, 0:n]),
                  reads=[bk[O2], "rec"], writes=[yfk])
            P.dma(lambda e, yf=yf, n=n, t0=t0: e.dma_start(out=yT[128:256, t0:t0 + n], in_=yf[:, 0:n]),
                  reads=[yfk], writes=[("yT", 1, ti)], q="sp")

        for ti, (qb0, nb) in enumerate(tiles):
            do_tile(ti, qb0, nb)
        P.emit()
    return nc


def build_T(first):
    nc = bass.Bass("TRN2", target_bir_lowering=False)
    io = {}
    def din(name, shape, dt=F32):
        io[name] = nc.dram_tensor(name, list(shape), dt, kind="ExternalInput").ap()
    if first:
        din("xin", [NT, D]); din("lng", [2, D])
    else:
        din("hprev", [NT, D]); din("hTp", [D, NT], BF16); din("yT", [D, NT], BF16)
        din("wg", [D, 2 * D]); din("wbs", [1024, D]); din("wbf", [1024, D]); din("wo", [D, D])
        din("lnm", [2, D]); din("wr", [D, NE]); din("rb", [1, NE])
        import os
        _ne = int(os.environ.get("T_NEXP", NE))
        din("wgate", [_ne, D, FF]); din("wup", [_ne, D, FF]); din("wdown", [_ne, FF, D]); din("lnf", [2, D])
    io["h"] = nc.dram_tensor("h", [NT, D], F32, kind="ExternalOutput").ap()
    io["hT"] = nc.dram_tensor("hT", [D, NT], BF16, kind="ExternalOutput").ap()
    emit_T(nc, io, first)
    return nc


def emit_T(nc, io, first):
    import contextlib
    with contextlib.ExitStack() as st:
        cx = Ctx(nc, st)
        P = Prog(nc)
        C = make_consts(P, cx, ["ident_f", "ident_b", "ustrict_b", "ones_b"])
        K = C["_key"]
        fb = [cx.ps([128, 512], F32, "bankf%d" % i) for i in range(6)]
        fk = ["bankf%d" % i for i in range(6)]
        tb = [cx.ps([128, 1024], BF16, "bankt%d" % i) for i in range(2)]
        tk = ["bankt%d" % i for i in range(2)]
        rr = [0]

        def nbank():
            rr[0] += 1
            return rr[0] % 6

        X = cx.sb([128, TB, D], F32, "X")
        XK = lambda b: ("X", b)
        st_s = cx.sb([128, 8], F32, "st_s")

        lncnt = [0]

        def layer_norm(gb_ap, cxl):
            lncnt[0] += 1
            G = cxl.sb([128, D], F32, "lnG%d" % lncnt[0])
            Bt = cxl.sb([128, D], F32, "lnB%d" % lncnt[0])
            junk = cxl.sb([128, D], BF16, "lnjunk%d" % lncnt[0])
            P.dma(lambda e: e.dma_start(out=G[:], in_=gb_ap[0:1, :].partition_broadcast(128)), writes=["lnG"])
            P.dma(lambda e: e.dma_start(out=Bt[:], in_=gb_ap[1:2, :].partition_broadcast(128)), writes=["lnB"])
            for b in range(TB):
                xs = X[:, b, :]
                P.dve(lambda e, xs=xs: e.reduce_sum(out=st_s[:, 0:1], in_=xs, axis=AX.X), reads=[XK(b)], writes=["st_s"])
                P.dve(lambda e: e.memset(st_s[:, 1:2], 0.0), writes=["st_s1"])
                P.act(lambda e, xs=xs: e.activation(out=junk[:], in_=xs, func=AF.Square, accum_out=st_s[:, 1:2]),
                      reads=[XK(b), "st_s1"], writes=["st_s1", "lnjunk"])
                P.dve(lambda e: e.tensor_scalar(out=st_s[:, 2:3], in0=st_s[:, 0:1], scalar1=1.0 / D, scalar2=None,
                                                op0=ALU.mult), reads=["st_s"], writes=["st_s2"])
                P.dve(lambda e: e.tensor_mul(out=st_s[:, 3:4], in0=st_s[:, 2:3], in1=st_s[:, 2:3]),
                      reads=["st_s2"], writes=["st_s3"])
                P.dve(lambda e: e.scalar_tensor_tensor(out=st_s[:, 4:5], in0=st_s[:, 1:2], scalar=1.0 / D,
                                                       in1=st_s[:, 3:4], op0=ALU.mult, op1=ALU.subtract),
                      reads=["st_s1", "st_s3"], writes=["st_s4"])
                P.act(lambda e: e.activation(out=st_s[:, 5:6], in_=st_s[:, 4:5], func=AF.Sqrt, bias=EPS_T[:, 0:1], scale=1.0),
                      reads=["st_s4", "eps"], writes=["st_s5"])
                P.dve(lambda e: e.reciprocal(out=st_s[:, 6:7], in_=st_s[:, 5:6]), reads=["st_s5"], writes=["st_s6"])
                P.dve(lambda e, xs=xs: e.tensor_scalar(out=xs, in0=xs, scalar1=st_s[:, 2:3], scalar2=st_s[:, 6:7],
                                                       op0=ALU.subtract, op1=ALU.mult),
                      reads=[XK(b), "st_s2", "st_s6"], writes=[XK(b)])
                P.dve(lambda e, xs=xs: e.tensor_mul(out=xs, in0=xs, in1=G[:]), reads=[XK(b), "lnG"], writes=[XK(b)])
                P.pool(lambda e, xs=xs: e.tensor_add(out=xs, in0=xs, in1=Bt[:]), reads=[XK(b), "lnB"], writes=[XK(b)])

        EPS_T = cx.sb([128, 1], F32, "eps")
        P.pool(lambda e: e.memset(EPS_T[:], EPS), writes=["eps"])

        def write_outputs(Hb, cxl):
            hv = io["h"].rearrange("(b p) d -> p b d", p=128)
            hTv = io["hT"].rearrange("(c p) t -> p c t", p=128)
            hts = [cxl.sb([128, 16, 128], BF16, "hts%d" % i) for i in range(2)]
            for b in range(TB):
                P.dma(lambda e, b=b: e.dma_start(out=hv[:, b, :], in_=X[:, b, :]), reads=[XK(b)], writes=[("hout", b)])
                P.act(lambda e, b=b: e.activation(out=Hb[:, b, :], in_=X[:, b, :], func=AF.Copy),
                      reads=[XK(b)], writes=[("Hb", b)])
                hs = hts[b % 2]
                hk = "hts%d" % (b % 2)
                for half in range(2):
                    t = tb[half]
                    for c in range(8):
                        cc = half * 8 + c
                        P.pe(lambda e, t=t, c=c, cc=cc, b=b: e.transpose(t[:, c * 128:(c + 1) * 128],
                                                                       Hb[:, b, cc * 128:(cc + 1) * 128], C["ident_b"][:]),
                             reads=[("Hb", b), K["ident_b"]], writes=[tk[half]])
                    if half == 0:
                        P.dve(lambda e, t=t, hs=hs: e.tensor_copy(out=hs[:, 0:8, :], in_=t[:].rearrange("p (c t) -> p c t", t=128)),
                              reads=[tk[half]], writes=[(hk, 0)])
                    else:
                        P.act(lambda e, t=t, hs=hs: e.activation(out=hs[:, 8:16, :], in_=t[:].rearrange("p (c t) -> p c t", t=128),
                                                                func=AF.Copy), reads=[tk[half]], writes=[(hk, 1)])
                P.dma(lambda e, b=b, hs=hs: e.dma_start(out=hTv[:, :, b * 128:(b + 1) * 128], in_=hs[:]),
                      reads=[(hk, 0), (hk, 1)], writes=[("hTout", b)])

        if first:
            xv = io["xin"].rearrange("(b p) d -> p b d", p=128)
            for b in range(TB):
                P.dma(lambda e, b=b: e.dma_start(out=X[:, b, :], in_=xv[:, b, :]), writes=[XK(b)])
            Hb = cx.sb([128, TB, D], BF16, "Hb")
            layer_norm(io["lng"], cx)
            write_outputs(Hb, cx)
            P.emit()
            return

        stA = contextlib.ExitStack()
        cxA = Ctx(nc, stA)
        MT = cxA.sb([128, 16, NT], BF16, "MT")
        stA1 = contextlib.ExitStack()
        cxA1 = Ctx(nc, stA1)
        Xb = X[:].rearrange("p b d -> p (b d)").bitcast(BF16)
        hTp = Xb[:, 0:16 * NT].rearrange("p (c t) -> p c t", t=NT)
        yTs = Xb[:, 16 * NT:32 * NT].rearrange("p (c t) -> p c t", t=NT)
        hv_ = io["hTp"].rearrange("(c p) t -> p c t", p=128)
        yv_ = io["yT"].rearrange("(c p) t -> p c t", p=128)
        for c4 in range(4):
            P.dma(lambda e, c4=c4: e.dma_start(out=hTp[:, 4 * c4:4 * c4 + 4, :], in_=hv_[:, 4 * c4:4 * c4 + 4, :]),
                  writes=[("hTp", c4)])
            P.dma(lambda e, c4=c4: e.dma_start(out=yTs[:, 4 * c4:4 * c4 + 4, :], in_=yv_[:, 4 * c4:4 * c4 + 4, :]),
                  writes=[("yTs", c4)])
        wgs = [cxA1.sb([128, 16, 128], BF16, "wgs%d" % i) for i in range(2)]
        wgf = [cxA1.sb([128, 16, 128], BF16, "wgf%d" % i) for i in range(2)]
        wbs = [cxA1.sb([128, 8, 128], BF16, "wbs%d" % i) for i in range(2)]
        wbf = [cxA1.sb([128, 8, 128], BF16, "wbf%d" % i) for i in range(2)]
        sgs = [cxA1.sb([128, 384], F32, "sgs%d" % i) for i in range(2)]
        sgf = [cxA1.sb([128, 384], F32, "sgf%d" % i) for i in range(2)]
        m1 = [cxA1.sb([128, 384], F32, "m1_%d" % i) for i in range(2)]
        m2 = [cxA1.sb([128, 384], F32, "m2_%d" % i) for i in range(2)]
        wgv = io["wg"].rearrange("(c p) n -> p c n", p=128)
        wbsv = io["wbs"].rearrange("(c p) n -> p c n", p=128)
        wbfv = io["wbf"].rearrange("(c p) n -> p c n", p=128)
        it = 0
        for nci in range(16):
            w = nci % 2
            n0 = nci * 128
            for hf in range(2):
                P.dma(lambda e, w=w, n0=n0, hf=hf: e.dma_start(out=wgs[w][:, 8 * hf:8 * hf + 8, :],
                                                              in_=wgv[:, 8 * hf:8 * hf + 8, n0:n0 + 128]),
                      writes=[("wgs", w, hf)], q="pool")
                P.dma(lambda e, w=w, n0=n0, hf=hf: e.dma_start(out=wgf[w][:, 8 * hf:8 * hf + 8, :],
                                                              in_=wgv[:, 8 * hf:8 * hf + 8, D + n0:D + n0 + 128]),
                      writes=[("wgf", w, hf)], q="pool")
            P.dma(lambda e, w=w, n0=n0: e.dma_start(out=wbs[w][:], in_=wbsv[:, :, n0:n0 + 128]), writes=[("wbs", w)], q="pool")
            P.dma(lambda e, w=w, n0=n0: e.dma_start(out=wbf[w][:], in_=wbfv[:, :, n0:n0 + 128]), writes=[("wbf", w)], q="pool")
            for tr in range(3):
                ts = slice(tr * 384, (tr + 1) * 384)
                u = it % 2
                it += 1
                bg, bb_, bg2, bb2 = nbank(), nbank(), nbank(), nbank()
                for c in range(16):
                    P.pe(lambda e, c=c, w=w, ts=ts, bg=bg: e.matmul(fb[bg][:, 0:384], wgs[w][:, c, :], hTp[:, c, ts],
                                                                    start=(c == 0), stop=(c == 15)),
                         reads=[("wgs", w, c // 8), ("hTp", c // 4)], writes=[fk[bg]])
                for c in range(8):
                    P.pe(lambda e, c=c, w=w, ts=ts, bb_=bb_: e.matmul(fb[bb_][:, 0:384], wbs[w][:, c, :], yTs[:, c, ts],
                                                                      start=(c == 0), stop=(c == 7)),
                         reads=[("wbs", w), ("yTs", c // 4)], writes=[fk[bb_]])
                for c in range(16):
                    P.pe(lambda e, c=c, w=w, ts=ts, bg2=bg2: e.matmul(fb[bg2][:, 0:384], wgf[w][:, c, :], hTp[:, c, ts],
                                                                      start=(c == 0), stop=(c == 15)),
                         reads=[("wgf", w, c // 8), ("hTp", c // 4)], writes=[fk[bg2]])
                for c in range(8):
                    P.pe(lambda e, c=c, w=w, ts=ts, bb2=bb2: e.matmul(fb[bb2][:, 0:384], wbf[w][:, c, :], yTs[:, 8 + c, ts],
                                                                      start=(c == 0), stop=(c == 7)),
                         reads=[("wbf", w), ("yTs", 2 + c // 4)], writes=[fk[bb2]])
                P.act(lambda e, u=u, bg=bg: e.activation(out=sgs[u][:], in_=fb[bg][:, 0:384], func=AF.Sigmoid),
                      reads=[fk[bg]], writes=[("sgs", u)])
                P.act(lambda e, u=u, bg2=bg2: e.activation(out=sgf[u][:], in_=fb[bg2][:, 0:384], func=AF.Sigmoid),
                      reads=[fk[bg2]], writes=[("sgf", u)])
                P.dve(lambda e, u=u, bb_=bb_: e.tensor_mul(out=m1[u][:], in0=sgs[u][:], in1=fb[bb_][:, 0:384]),
                      reads=[("sgs", u), fk[bb_]], writes=[("m1", u)])
                P.dve(lambda e, u=u, bb2=bb2: e.tensor_mul(out=m2[u][:], in0=sgf[u][:], in1=fb[bb2][:, 0:384]),
                      reads=[("sgf", u), fk[bb2]], writes=[("m2", u)])
                P.pool(lambda e, u=u, nci=nci, ts=ts: e.tensor_add(out=MT[:, nci, ts], in0=m1[u][:], in1=m2[u][:]),
                       reads=[("m1", u), ("m2", u)], writes=[("MT", nci)])
        import os
        _stop = int(os.environ.get("T_STOP", "9"))
        if _stop <= 1:
            P.emit(); stA1.close(); stA.close(); return
        P.barrier()
        stA1.close()

        stA2 = contextlib.ExitStack()
        cxA2 = Ctx(nc, stA2)
        hpv = io["hprev"].rearrange("(b p) d -> p b d", p=128)
        for b in range(TB):
            P.dma(lambda e, b=b: e.dma_start(out=X[:, b, :], in_=hpv[:, b, :]), writes=[XK(b)])
        woc = [cxA2.sb([128, 16, 512], BF16, "woc%d" % i) for i in range(2)]
        wov = io["wo"].rearrange("(c p) n -> p c n", p=128)
        for cg in range(4):
            w = cg % 2
            for q4 in range(4):
                P.dma(lambda e, w=w, cg=cg, q4=q4: e.dma_start(out=woc[w][:, 4 * q4:4 * q4 + 4, :],
                                                              in_=wov[:, 4 * q4:4 * q4 + 4, cg * 512:(cg + 1) * 512]),
                      writes=[("woc", w, q4)], q="pool")
            for b in range(TB):
                bo = nbank()
                for c in range(16):
                    P.pe(lambda e, c=c, w=w, b=b, bo=bo: e.matmul(fb[bo][:, :], MT[:, c, b * 128:(b + 1) * 128], woc[w][:, c, :],
                                                                  start=(c == 0), stop=(c == 15)),
                         reads=[("MT", c), ("woc", w, c // 4)], writes=[fk[bo]])
                P.dve(lambda e, b=b, cg=cg, bo=bo: e.scalar_tensor_tensor(
                    out=X[:, b, cg * 512:(cg + 1) * 512], in0=X[:, b, cg * 512:(cg + 1) * 512], scalar=ALPHA,
                    in1=fb[bo][:, :], op0=ALU.mult, op1=ALU.add), reads=[XK(b), fk[bo]], writes=[XK(b)])
        layer_norm(io["lnm"], cxA2)
        if _stop <= 2:
            P.emit(); stA2.close(); stA.close(); return
        P.barrier()
        stA2.close()
        stA.close()

        Hb = cx.sb([128, TB, D], BF16, "Hb")
        RT = cx.sb([128, TB, NE], F32, "aff")
        MSK = cx.sb([128, TB, NE], F32, "msk")
        WN = cx.sb([128, TB, NE], F32, "wn")
        POS = cx.sb([128, TB, NE], F32, "pos")
        WHL = cx.sb([128, TB, NE, 2], BF16, "whl")
        iota_f = cx.sb([128, CAP], F32, "iota_f")
        iota_i = cx.sb([128, CAP], I32, "iota_i")
        P.pool(lambda e: e.iota(iota_i[:], pattern=[[1, CAP]], base=0, channel_multiplier=0), writes=["iota_i"])
        P.pool(lambda e: e.tensor_copy(out=iota_f[:], in_=iota_i[:]), reads=["iota_i"], writes=["iota_f"])
        stC = contextlib.ExitStack()
        cxC = Ctx(nc, stC)
        wr_sb = cxC.sb([128, 16, NE], F32, "wr_sb")
        rb_sb = cxC.sb([128, NE], F32, "rb_sb")
        wrv = io["wr"].rearrange("(c p) e -> p c e", p=128)
        for c4 in range(4):
            P.dma(lambda e, c4=c4: e.dma_start(out=wr_sb[:, 4 * c4:4 * c4 + 4, :], in_=wrv[:, 4 * c4:4 * c4 + 4, :]),
                  writes=[("wr", c4)])
        P.dma(lambda e: e.dma_start(out=rb_sb[:], in_=io["rb"].partition_broadcast(128)), writes=["rb"])
        xT32 = [cxC.sb([128, 16, 128], F32, "xT32_%d" % i) for i in range(2)]
        for b in range(TB):
            P.act(lambda e, b=b: e.activation(out=Hb[:, b, :], in_=X[:, b, :], func=AF.Copy), reads=[XK(b)], writes=[("Hb", b)])
            xt = xT32[b % 2]
            xk = "xT32_%d" % (b % 2)
            for q4 in range(4):
                bt_ = nbank()
                for c in range(4):
                    cc = q4 * 4 + c
                    P.pe(lambda e, b=b, c=c, cc=cc, bt_=bt_: e.transpose(fb[bt_][:, c * 128:(c + 1) * 128],
                                                                        X[:, b, cc * 128:(cc + 1) * 128], C["ident_f"][:]),
                         reads=[XK(b), K["ident_f"]], writes=[fk[bt_]])
                if q4 % 2 == 0:
                    P.dve(lambda e, xt=xt, q4=q4, bt_=bt_: e.tensor_copy(out=xt[:, 4 * q4:4 * q4 + 4, :],
                                                                        in_=fb[bt_][:].rearrange("p (c t) -> p c t", t=128)),
                          reads=[fk[bt_]], writes=[(xk, q4)])
                else:
                    P.act(lambda e, xt=xt, q4=q4, bt_=bt_: e.activation(out=xt[:, 4 * q4:4 * q4 + 4, :],
                                                                       in_=fb[bt_][:].rearrange("p (c t) -> p c t", t=128),
                                                                       func=AF.Copy), reads=[fk[bt_]], writes=[(xk, q4)])
            br = nbank()
            for c in range(16):
                P.pe(lambda e, c=c, xt=xt, br=br: e.matmul(fb[br][:, 0:NE], xt[:, c, :], wr_sb[:, c, :],
                                                           start=(c == 0), stop=(c == 15)),
                     reads=[(xk, c // 4), ("wr", c // 4)], writes=[fk[br]])
            P.act(lambda e, b=b, br=br: e.activation(out=RT[:, b, :], in_=fb[br][:, 0:NE], func=AF.Sigmoid),
                  reads=[fk[br]], writes=[("aff", b)])
        for b in range(TB):
            P.pool(lambda e, b=b: e.tensor_scalar(out=X[:, b, :], in0=X[:, b, :], scalar1=ALPHA, scalar2=None, op0=ALU.mult),
                   reads=[XK(b)], writes=[XK(b)])
        affk = [("aff", b) for b in range(TB)]
        sel = cxC.sb([128, TB, NE], F32, "sel")
        sel2 = cxC.sb([128, TB, NE], F32, "sel2")
        eq = cxC.sb([128, TB, NE], F32, "eq")
        mx1 = cxC.sb([128, TB * 4], F32, "mx1")
        mx2 = cxC.sb([128, TB * 4], F32, "mx2")
        gs = cxC.sb([128, TB, 4], F32, "gs")
        gmax = cxC.sb([128, TB], F32, "gmax")
        best = cxC.sb([128, TB * 4], F32, "best")
        wsum = cxC.sb([128, TB], F32, "wsum")
        tmpw = cxC.sb([128, TB, NE], F32, "tmpw")
        for b in range(TB):
            P.dve(lambda e, b=b: e.tensor_add(out=sel[:, b, :], in0=RT[:, b, :], in1=rb_sb[:]),
                  reads=[("aff", b), "rb"], writes=["sel"])
        s4 = lambda t: t[:].rearrange("p b (g k) -> p (b g) k", k=4)
        bc = lambda t: t[:].unsqueeze(2).to_broadcast([128, TB * 4, 4])
        P.dve(lambda e: e.tensor_reduce(out=mx1[:], in_=s4(sel), axis=AX.X, op=ALU.max), reads=["sel"], writes=["mx1"])
        P.dve(lambda e: e.tensor_tensor(out=s4(eq), in0=s4(sel), in1=bc(mx1), op=ALU.is_equal),
              reads=["sel", "mx1"], writes=["eq"])
        P.dve(lambda e: e.scalar_tensor_tensor(out=sel2[:], in0=eq[:], scalar=-1e9, in1=sel[:], op0=ALU.mult, op1=ALU.add),
              reads=["eq", "sel"], writes=["sel2"])
        P.dve(lambda e: e.tensor_reduce(out=mx2[:], in_=s4(sel2), axis=AX.X, op=ALU.max), reads=["sel2"], writes=["mx2"])
        P.dve(lambda e: e.tensor_add(out=gs[:].rearrange("p b g -> p (b g)"), in0=mx1[:], in1=mx2[:]),
              reads=["mx1", "mx2"], writes=["gs"])
        P.dve(lambda e: e.tensor_reduce(out=gmax[:], in_=gs[:], axis=AX.X, op=ALU.max), reads=["gs"], writes=["gmax"])
        P.dve(lambda e: e.tensor_tensor(out=best[:].rearrange("p (b g) -> p b g", g=4), in0=gs[:],
                                        in1=gmax[:].unsqueeze(2).to_broadcast([128, TB, 4]), op=ALU.is_equal),
              reads=["gs", "gmax"], writes=["best"])
        P.dve(lambda e: e.tensor_tensor(out=s4(eq), in0=s4(sel), in1=bc(mx2), op=ALU.is_ge),
              reads=["sel", "mx2", "sel2"], writes=["eq"])
        P.dve(lambda e: e.tensor_tensor(out=s4(MSK), in0=s4(eq), in1=bc(best), op=ALU.mult),
              reads=["eq", "best"], writes=["msk"])
        P.dve(lambda e: e.memset(MSK[0:NPAD, 0, :], 0.0), reads=["msk"], writes=["msk"])
        P.dve(lambda e: e.tensor_mul(out=tmpw[:], in0=MSK[:], in1=RT[:]), reads=["msk"] + affk, writes=["tmpw"])
        P.dve(lambda e: e.tensor_reduce(out=wsum[:], in_=tmpw[:], axis=AX.X, op=ALU.add), reads=["tmpw"], writes=["wsum"])
        P.dve(lambda e: e.tensor_scalar(out=wsum[:], in0=wsum[:], scalar1=1e-30, scalar2=None, op0=ALU.add),
              reads=["wsum"], writes=["wsum"])
        P.dve(lambda e: e.reciprocal(out=wsum[:], in_=wsum[:]), reads=["wsum"], writes=["wsum"])
        P.dve(lambda e: e.tensor_tensor(out=WN[:], in0=tmpw[:], in1=wsum[:].unsqueeze(2).to_broadcast([128, TB, NE]),
                                        op=ALU.mult), reads=["tmpw", "wsum"], writes=["wn"])
        P.dve(lambda e: e.tensor_copy(out=WHL[:, :, :, 0], in_=WN[:]), reads=["wn"], writes=["whl0"])
        P.dve(lambda e: e.tensor_copy(out=tmpw[:], in_=WHL[:, :, :, 0]), reads=["whl0", "wn"], writes=["tmpw"])
        P.dve(lambda e: e.tensor_sub(out=tmpw[:], in0=WN[:], in1=tmpw[:]), reads=["wn", "tmpw"], writes=["tmpw"])
        P.dve(lambda e: e.tensor_copy(out=WHL[:, :, :, 1], in_=tmpw[:]), reads=["tmpw"], writes=["whl1"])
        mskb = cxC.sb([128, TB, NE], BF16, "mskb")
        cmb = cxC.sb([128, TB, NE], BF16, "cmb")
        P.dve(lambda e: e.tensor_copy(out=mskb[:], in_=MSK[:]), reads=["msk"], writes=["mskb"])
        P.dve(lambda e: e.tensor_copy(out=cmb[:, 0, :], in_=mskb[:, 0, :]), reads=["mskb"], writes=[("cmb", 0)])
        for b in range(1, TB):
            P.dve(lambda e, b=b: e.tensor_add(out=cmb[:, b, :], in0=cmb[:, b - 1, :], in1=mskb[:, b, :]),
                  reads=["mskb", ("cmb", b - 1)], writes=[("cmb", b)])
        for b in range(TB):
            bp = nbank()
            P.pe(lambda e, b=b, bp=bp: e.matmul(fb[bp][:, 0:NE], C["ustrict_b"][:], mskb[:, b, :], start=True, stop=(b == 0)),
                 reads=["mskb", K["ustrict_b"]], writes=[fk[bp]])
            if b > 0:
                P.pe(lambda e, b=b, bp=bp: e.matmul(fb[bp][:, 0:NE], C["ones_b"][:], cmb[:, b - 1, :], start=False, stop=True),
                     reads=[("cmb", b - 1), K["ones_b"]], writes=[fk[bp]])
            P.dve(lambda e, b=b, bp=bp: e.tensor_copy(out=POS[:, b, :], in_=fb[bp][:, 0:NE]), reads=[fk[bp]], writes=["pos"])
        if _stop <= 3:
            P.emit(); stC.close(); return
        P.barrier()
        stC.close()

        stD = contextlib.ExitStack()
        cxD = Ctx(nc, stD)
        _S0 = cxD.sb([128, TB, CAP], BF16, "S0")
        S = [_S0, _S0]
        ST = [cxD.sb([128, 2, NT], BF16, "ST%d" % i) for i in range(2)]
        XeT2 = [cxD.sb([128, 16, CAP], BF16, "XeT%d" % i) for i in range(2)]
        AT = cxD.sb([128, 8, CAP], BF16, "AT")
        Yb = cxD.sb([128, 2, D], BF16, "Yb")
        wsl = [cxD.sb([128, 2], F32, "wsl%d" % i) for i in range(2)]
        sgl = [cxD.sb([128, CAP], F32, "sgl%d" % i) for i in range(2)]
        wgp = [cxD.sb([128, 16, 256], BF16, "wgp%d" % i) for i in range(2)]
        wup = [cxD.sb([128, 16, 256], BF16, "wup%d" % i) for i in range(2)]
        wdp = [cxD.sb([128, 8, 512], BF16, "wdp%d" % i) for i in range(2)]
        allX = [XK(b) for b in range(TB)]
        pcnt = [0]
        dcnt = [0]

        def part1(ex):
            s = ex % 2
            XeT = XeT2[s]
            Sk = lambda b: ("S", 0, b)
            for b in range(TB):
                P.dve(lambda e, b=b: e.tensor_scalar(out=S[s][:, b, :], in0=iota_f[:], scalar1=POS[:, b, ex:ex + 1],
                                                     scalar2=MSK[:, b, ex:ex + 1], op0=ALU.is_equal, op1=ALU.mult),
                      reads=["pos", "msk", "iota_f"], writes=[Sk(b)])
            bw = nbank()
            for jb in range(2):
                for b in range(TB):
                    P.pe(lambda e, jb=jb, b=b: e.matmul(fb[bw][:, 2 * jb:2 * jb + 2], S[s][:, b, jb * 128:(jb + 1) * 128],
                                                        WHL[:, b, ex, :], start=(jb == 0 and b == 0), stop=(b == TB - 1),
                                                        skip_group_check=True),
                         reads=[Sk(b), "whl0", "whl1"], writes=[fk[bw]])
            P.dve(lambda e: e.tensor_reduce(out=wsl[s][:], in_=fb[bw][:, 0:4].rearrange("p (j k) -> p j k", k=2),
                                            axis=AX.X, op=ALU.add), reads=[fk[bw]], writes=[("wsl", s)])
            for jb in range(2):
                for b in range(TB):
                    t = tb[0] if b < 8 else tb[1]
                    tkk = tk[0] if b < 8 else tk[1]
                    off = (b % 8) * 128
                    P.pe(lambda e, jb=jb, b=b, t=t, off=off: e.transpose(t[:, off:off + 128], S[s][:, b, jb * 128:(jb + 1) * 128],
                                                                        C["ident_b"][:]),
                         reads=[Sk(b), K["ident_b"]], writes=[tkk])
                P.dve(lambda e, jb=jb: e.tensor_copy(out=ST[s][:, jb, 0:1024], in_=tb[0][:, :]), reads=[tk[0]], writes=[("ST", s, jb, 0)])
                P.act(lambda e, jb=jb: e.activation(out=ST[s][:, jb, 1024:NT], in_=tb[1][:, 0:128], func=AF.Copy),
                      reads=[tk[1]], writes=[("ST", s, jb, 1)])
            for c in range(16):
                bgx = nbank()
                for b in range(TB):
                    P.pe(lambda e, c=c, b=b, bgx=bgx: e.matmul(fb[bgx][:, 0:CAP], Hb[:, b, c * 128:(c + 1) * 128], S[s][:, b, :],
                                                               start=(b == 0), stop=(b == TB - 1)),
                         reads=[("Hb", b), Sk(b)], writes=[fk[bgx]])
                if c % 2 == 0:
                    P.dve(lambda e, c=c, bgx=bgx: e.tensor_copy(out=XeT[:, c, :], in_=fb[bgx][:, 0:CAP]),
                          reads=[fk[bgx]], writes=[("XeT", s, c)])
                else:
                    P.act(lambda e, c=c, bgx=bgx: e.activation(out=XeT[:, c, :], in_=fb[bgx][:, 0:CAP], func=AF.Copy),
                          reads=[fk[bgx]], writes=[("XeT", s, c)])
        def part2(ex):
            s = ex % 2
            XeT = XeT2[s]
            gv = io["wgate"][ex].rearrange("(c p) f -> p c f", p=128)
            uv = io["wup"][ex].rearrange("(c p) f -> p c f", p=128)
            for pc in range(4):
                w = pcnt[0] % 2
                pcnt[0] += 1
                for hf in range(2):
                    P.dma(lambda e, w=w, pc=pc, hf=hf: e.dma_start(out=wgp[w][:, 8 * hf:8 * hf + 8, :],
                                                                  in_=gv[:, 8 * hf:8 * hf + 8, pc * 256:(pc + 1) * 256]),
                          writes=[("wgp", w, hf)], q="pool")
                    P.dma(lambda e, w=w, pc=pc, hf=hf: e.dma_start(out=wup[w][:, 8 * hf:8 * hf + 8, :],
                                                                  in_=uv[:, 8 * hf:8 * hf + 8, pc * 256:(pc + 1) * 256]),
                          writes=[("wup", w, hf)], q="pool")
                for fl in range(2):
                    fc = pc * 2 + fl
                    bg, bu = nbank(), nbank()
                    for c in range(16):
                        P.pe(lambda e, c=c, w=w, fl=fl, bg=bg: e.matmul(fb[bg][:, 0:CAP], wgp[w][:, c, fl * 128:(fl + 1) * 128],
                                                                        XeT[:, c, :], start=(c == 0), stop=(c == 15)),
                             reads=[("wgp", w, c // 8), ("XeT", s, c)], writes=[fk[bg]])
                    for c in range(16):
                        P.pe(lambda e, c=c, w=w, fl=fl, bu=bu: e.matmul(fb[bu][:, 0:CAP], wup[w][:, c, fl * 128:(fl + 1) * 128],
                                                                        XeT[:, c, :], start=(c == 0), stop=(c == 15)),
                             reads=[("wup", w, c // 8), ("XeT", s, c)], writes=[fk[bu]])
                    u = fc % 2
                    P.act(lambda e, u=u, bg=bg: e.activation(out=sgl[u][:], in_=fb[bg][:, 0:CAP], func=AF.Silu),
                          reads=[fk[bg]], writes=[("sgl", u)])
                    P.dve(lambda e, u=u, bu=bu, fc=fc: e.tensor_mul(out=AT[:, fc, :], in0=sgl[u][:], in1=fb[bu][:, 0:CAP]),
                          reads=[("sgl", u), fk[bu]], writes=[("AT", fc)])
        def part3(ex):
            s = ex % 2
            dv = io["wdown"][ex].rearrange("(c p) n -> p c n", p=128)
            ATk = [("AT", fc) for fc in range(8)]
            for cg in range(4):
                w = dcnt[0] % 2
                dcnt[0] += 1
                for hf in range(2):
                    P.dma(lambda e, w=w, cg=cg, hf=hf: e.dma_start(out=wdp[w][:, 4 * hf:4 * hf + 4, :],
                                                                  in_=dv[:, 4 * hf:4 * hf + 4, cg * 512:(cg + 1) * 512]),
                          writes=[("wdp", w, hf)], q="pool")
                for jb in range(2):
                    by = nbank()
                    for fc in range(8):
                        P.pe(lambda e, fc=fc, jb=jb, w=w, by=by: e.matmul(fb[by][:, :], AT[:, fc, jb * 128:(jb + 1) * 128],
                                                                          wdp[w][:, fc, :], start=(fc == 0), stop=(fc == 7)),
                             reads=[("AT", fc), ("wdp", w, fc // 4)], writes=[fk[by]])
                    if jb == 0:
                        P.act(lambda e, jb=jb, cg=cg, by=by: e.activation(out=Yb[:, jb, cg * 512:(cg + 1) * 512], in_=fb[by][:, :],
                                                                          func=AF.Copy, scale=wsl[s][:, jb:jb + 1]),
                              reads=[fk[by], ("wsl", s)], writes=[("Yb", jb, cg)])
                    else:
                        P.dve(lambda e, jb=jb, cg=cg, by=by: e.tensor_scalar(out=Yb[:, jb, cg * 512:(cg + 1) * 512], in0=fb[by][:, :],
                                                                             scalar1=wsl[s][:, jb:jb + 1], scalar2=None, op0=ALU.mult),
                              reads=[fk[by], ("wsl", s)], writes=[("Yb", jb, cg)])
        def part4(ex):
            s = ex % 2
            for cg in range(4):
                for b in range(TB):
                    bc_ = nbank()
                    for jb in range(2):
                        P.pe(lambda e, jb=jb, b=b, cg=cg, bc_=bc_: e.matmul(fb[bc_][:, :], ST[s][:, jb, b * 128:(b + 1) * 128],
                                                                            Yb[:, jb, cg * 512:(cg + 1) * 512],
                                                                            start=(jb == 0), stop=(jb == 1)),
                             reads=[("ST", s, jb, 0 if b < 8 else 1), ("Yb", jb, cg)], writes=[fk[bc_]])
                    P.dve(lambda e, b=b, cg=cg, bc_=bc_: e.tensor_add(out=X[:, b, cg * 512:(cg + 1) * 512],
                                                                      in0=X[:, b, cg * 512:(cg + 1) * 512], in1=fb[bc_][:, :]),
                          reads=[XK(b), fk[bc_]], writes=[XK(b)])

        import os
        _nex = int(os.environ.get("T_NEXP", NE))
        part1(0)
        for ex in range(_nex):
            part2(ex)
            if ex + 1 < _nex:
                part1(ex + 1)
            part3(ex)
            part4(ex)
        P.barrier()
        stD.close()

        layer_norm(io["lnf"], cx)
        write_outputs(Hb, cx)
        P.emit()


_PROGS = {}


def _prog(name):
    if name not in _PROGS:
        _PROGS[name] = {"T0": lambda: build_T(True), "A": build_A, "T": lambda: build_T(False)}[name]()
    return _PROGS[name]


def _run(name, in_maps):
    res = run_bass_kernel_spmd(_prog(name), in_maps, core_ids=list(range(NCORE)))
    return res.results


def _gather_hT(outs):
    full = np.empty((D, L), dtype=ml_dtypes.bfloat16)
    full[:, 0:128] = np.asarray(outs[0]["hT"])[:, 0:128]
    for c in range(NCORE):
        full[:, 128 + 1024 * c:128 + 1024 * (c + 1)] = np.asarray(outs[c]["hT"])[:, 128:NT]
    return full


def kernel(x, meta_tokens, ln_in_g, ln_in_b, w_in, b_forget, w_branch_sb, w_branch_fox, w_out,
           ln_mix_g, ln_mix_b, w_router, router_bias, w_gate, w_up, w_down, ln_ffn_g, ln_ffn_b):
    f32 = np.float32
    x = np.asarray(x, f32)
    w_in = np.asarray(w_in, f32)
    depth = w_in.shape[0]
    blk0 = np.concatenate([np.zeros((NPAD, D), f32), np.asarray(meta_tokens, f32)], 0)
    lng = np.stack([np.asarray(ln_in_g, f32), np.asarray(ln_in_b, f32)])
    t_in = [{"xin": np.ascontiguousarray(np.concatenate([blk0, x[0, 1024 * c:1024 * (c + 1)]], 0)), "lng": lng}
            for c in range(NCORE)]
    outs = _run("T0", t_in)
    cols = [np.concatenate([np.arange(0, 128), 128 + 1024 * c + np.arange(1024)]) for c in range(NCORE)]
    for i in range(depth):
        hT_all = _gather_hT(outs)
        a_in = []
        for c in range(NCORE):
            s = slice(c * 128, (c + 1) * 128)
            wA = np.concatenate([w_in[i][:, 0:1024][:, s], w_in[i][:, 1024:2048][:, s],
                                 w_in[i][:, 3072:4096][:, s], w_in[i][:, 4096:5120][:, s],
                                 w_in[i][:, 2048:3072][:, s], w_in[i][:, 5120:6144][:, s],
                                 w_in[i][:, 6144 + c:6145 + c]], 1)
            a_in.append({"hT": hT_all, "wA": np.ascontiguousarray(wA),
                         "bf": np.asarray(b_forget, f32)[i, c].reshape(1, 1)})
        a_out = _run("A", a_in)
        del a_in, hT_all
        Y = np.empty((D, L), dtype=ml_dtypes.bfloat16)
        for c in range(NCORE):
            y = np.asarray(a_out[c]["yT"])
            Y[c * 128:(c + 1) * 128] = y[0:128]
            Y[1024 + c * 128:1024 + (c + 1) * 128] = y[128:256]
        wg = np.ascontiguousarray(w_in[i][:, 6152:6152 + 2 * D])
        shared = {"wg": wg, "wbs": np.asarray(w_branch_sb, f32)[i], "wbf": np.asarray(w_branch_fox, f32)[i],
                  "wo": np.asarray(w_out, f32)[i],
                  "lnm": np.stack([np.asarray(ln_mix_g, f32)[i], np.asarray(ln_mix_b, f32)[i]]),
                  "wr": np.asarray(w_router, f32), "rb": np.asarray(router_bias, f32).reshape(1, NE),
                  "wgate": np.asarray(w_gate, f32)[i], "wup": np.asarray(w_up, f32)[i],
                  "wdown": np.asarray(w_down, f32)[i],
                  "lnf": np.stack([np.asarray(ln_ffn_g, f32)[i], np.asarray(ln_ffn_b, f32)[i]])}
        t_in = []
        for c in range(NCORE):
            m = dict(shared)
            m["hprev"] = np.asarray(outs[c]["h"])
            m["hTp"] = np.asarray(outs[c]["hT"])
            m["yT"] = np.ascontiguousarray(Y[:, cols[c]])
            t_in.append(m)
        outs = _run("T", t_in)
        del t_in, Y
    out = np.empty((1, SEQ, D), f32)
    for c in range(NCORE):
        out[0, 1024 * c:1024 * (c + 1)] = np.asarray(outs[c]["h"])[128:NT]
    return out
```

```python
import numpy as np
import ml_dtypes
import concourse.bass as bass
import concourse.mybir as mybir
from concourse.bass_utils import run_bass_kernel_spmd

F32 = mybir.dt.float32
BF16 = mybir.dt.bfloat16
I32 = mybir.dt.int32
AF = mybir.ActivationFunctionType
ALU = mybir.AluOpType
AX = mybir.AxisListType

D = 2048
SEQ = 8192
L = SEQ + 128
NBLK = L // 128
DH = 128
NPAD = 112
NE = 16
FF = 1024
CAP = 256
ALPHA = 4.0 ** 0.25
EPS = 1e-5
NCORE = 8
TB = 9
NT = TB * 128


class Prog:
    SELFSYNC = ("act", "dve", "pool")

    def __init__(self, nc, n_dma_sems=12):
        self.nc = nc
        self.ops = []
        self.res_w = {}
        self.res_r = {}
        self.n_dma_sems = n_dma_sems

    def add(self, eng, fn, reads=(), writes=(), dma=False):
        idx = len(self.ops)
        deps = set()
        ex = [r for r in reads if isinstance(r, str) and r.startswith("bank")]
        if ex:
            reads = [r for r in reads if r not in ex]
            writes = list(writes) + [r for r in ex if r not in writes]
        for r in reads:
            w = self.res_w.get(r)
            if w is not None:
                deps.add(w)
        for w_ in writes:
            w = self.res_w.get(w_)
            if w is not None:
                deps.add(w)
            for rd in self.res_r.get(w_, ()):
                deps.add(rd)
        deps.discard(idx)
        self.ops.append(dict(eng=eng, fn=fn, deps=deps, dma=dma, sig=dma))
        for r in reads:
            self.res_r.setdefault(r, []).append(idx)
        for w_ in writes:
            self.res_w[w_] = idx
            self.res_r[w_] = []
        return idx

    def pe(self, fn, reads=(), writes=()):
        return self.add("pe", fn, reads, writes)

    def act(self, fn, reads=(), writes=()):
        return self.add("act", fn, reads, writes)

    def dve(self, fn, reads=(), writes=()):
        return self.add("dve", fn, reads, writes)

    def pool(self, fn, reads=(), writes=()):
        return self.add("pool", fn, reads, writes)

    def dma(self, fn, reads=(), writes=(), q="sp"):
        return self.add(q, fn, reads, writes, dma=True)

    def barrier(self):
        last = {}
        dmas = set()
        for i, x in enumerate(self.ops):
            if x["fn"] is None:
                continue
            if x["dma"]:
                dmas.add(i)
            else:
                last[x["eng"]] = i
        deps = set(last.values()) | dmas
        for e in ("pe", "act", "dve", "pool", "sp"):
            self.ops.append(dict(eng=e, fn=None, deps=set(deps), dma=False, sig=False, bar=True))

    def emit(self, final_waits=True):
        nc = self.nc
        ops = self.ops
        for x in ops:
            for d in x["deps"]:
                dd = ops[d]
                if dd["dma"]:
                    continue
                if dd["eng"] != x["eng"] or x["dma"] or dd["eng"] in self.SELFSYNC or x.get("bar"):
                    dd["sig"] = True
        engs = ["pe", "act", "dve", "pool", "sp"]
        import contextlib
        with contextlib.ExitStack() as st:
            esem = {e: st.enter_context(nc.semaphore("sem_" + e)) for e in engs}
            dsem = {q: [st.enter_context(nc.semaphore("dsem_%s_%d" % (q, j)))
                        for j in range(self.n_dma_sems)] for q in ("sp", "pool", "act")}
            ecount = {e: 0 for e in engs}
            dnext = {q: 0 for q in dsem}
            duse = {q: [0] * self.n_dma_sems for q in dsem}
            for x in ops:
                if x["dma"]:
                    q = x["eng"]
                    j = dnext[q]
                    dnext[q] = (j + 1) % self.n_dma_sems
                    duse[q][j] += 1
                    x["sem"] = dsem[q][j]
                    x["val"] = 16 * duse[q][j]
                    x["prev"] = 16 * (duse[q][j] - 1)
                elif x["sig"]:
                    ecount[x["eng"]] += 1
                    x["sem"] = esem[x["eng"]]
                    x["val"] = ecount[x["eng"]]
            block = st.enter_context(nc.Block())
            per_eng = {e: [x for x in ops if x["eng"] == e] for e in engs}
            last_dma = [x for x in ops if x["dma"]]

            def run(e, eng):
                waited = {}

                def wait(sem, val):
                    key = id(sem)
                    if waited.get(key, 0) >= val:
                        return
                    waited[key] = val
                    eng.wait_ge(sem, val)

                for x in per_eng[e]:
                    for d in sorted(x["deps"]):
                        dd = ops[d]
                        if (not dd["dma"]) and dd["eng"] == e and not x["dma"] and e not in self.SELFSYNC \
                                and not x.get("bar"):
                            continue
                        wait(dd["sem"], dd["val"])
                    if x["fn"] is None:
                        continue
                    if x["dma"] and x["prev"] > 0:
                        wait(x["sem"], x["prev"])
                    ins = x["fn"](eng)
                    if x["dma"]:
                        ins.then_inc(x["sem"], 16)
                    elif x["sig"]:
                        ins.then_inc(x["sem"], 1)
                if e == "sp" and final_waits:
                    for q in dsem:
                        for j in range(self.n_dma_sems):
                            if duse[q][j] > 0:
                                eng.wait_ge(dsem[q][j], 16 * duse[q][j])

            @block.tensor
            def _(eng):
                run("pe", eng)

            @block.scalar
            def _(eng):
                run("act", eng)

            @block.vector
            def _(eng):
                run("dve", eng)

            @block.gpsimd
            def _(eng):
                run("pool", eng)

            @block.sync
            def _(eng):
                run("sp", eng)


class Ctx:
    def __init__(self, nc, stack):
        self.nc = nc
        self.stack = stack
        self.n = 0

    def sb(self, shape, dt, name=None):
        self.n += 1
        return self.stack.enter_context(self.nc.sbuf_tensor(name or ("t%d" % self.n), list(shape), dt))

    def ps(self, shape, dt=F32, name=None):
        self.n += 1
        return self.stack.enter_context(self.nc.psum_tensor(name or ("p%d" % self.n), list(shape), dt))


def make_consts(P, cx, names):
    c = {}

    def tri(name, cmp_op, dt, val, flip=False, n=128):
        sgn = -1 if flip else 1
        tf = cx.sb([128, n], F32, name + "_f")
        P.pool(lambda e: e.memset(tf[:], val), writes=[name + "_f"])
        P.pool(lambda e: e.affine_select(out=tf[:], in_=tf[:], pattern=[[sgn, n]], compare_op=cmp_op,
                                         fill=0.0, base=0, channel_multiplier=-sgn),
               reads=[name + "_f"], writes=[name + "_f"])
        if dt == F32:
            c[name] = tf
            return name + "_f"
        tb = cx.sb([128, n], dt, name)
        P.pool(lambda e: e.tensor_copy(out=tb[:], in_=tf[:]), reads=[name + "_f"], writes=[name])
        c[name] = tb
        return name

    spec = {
        "negge": (ALU.is_ge, BF16, -1.0, True),
        "neglt": (ALU.is_gt, BF16, -1.0),
        "triincl": (ALU.is_ge, F32, 1.0),
        "ustrict": (ALU.is_gt, F32, 1.0),
        "ustrict_b": (ALU.is_gt, BF16, 1.0),
        "mstrict": (ALU.is_gt, F32, 1.0),
        "mincl": (ALU.is_ge, BF16, 1.0),
        "ident_f": (ALU.is_equal, F32, 1.0),
        "ident_b": (ALU.is_equal, BF16, 1.0),
    }
    c["_key"] = {}
    for nm in names:
        if nm in spec:
            c["_key"][nm] = tri(nm, *spec[nm])
        elif nm == "ones_b":
            t = cx.sb([128, 128], BF16, "ones_b")
            P.pool(lambda e, t=t: e.memset(t[:], 1.0), writes=["ones_b"])
            c[nm] = t
            c["_key"][nm] = "ones_b"
        elif nm == "ones_f":
            t = cx.sb([128, 128], F32, "ones_f")
            P.pool(lambda e, t=t: e.memset(t[:], 1.0), writes=["ones_f"])
            c[nm] = t
            c["_key"][nm] = "ones_f"
    return c


def build_A(upto=3):
    import contextlib
    nc = bass.Bass("TRN2", target_bir_lowering=False)
    hT = nc.dram_tensor("hT", [D, L], BF16, kind="ExternalInput").ap()
    wA = nc.dram_tensor("wA", [D, 769], F32, kind="ExternalInput").ap()
    bfg = nc.dram_tensor("bf", [1, 1], F32, kind="ExternalInput").ap()
    yT = nc.dram_tensor("yT", [256, L], BF16, kind="ExternalOutput").ap()
    cscr = nc.dram_tensor("cscr", [6, L], BF16, kind="Internal").ap()
    emit_A(nc, hT, wA, bfg, yT, cscr, upto)
    return nc


def emit_A(nc, hT, wA, bfg, yT, cscr, upto=3):
    import contextlib
    with contextlib.ExitStack() as st:
        cx = Ctx(nc, st)
        P = Prog(nc)
        C = make_consts(P, cx, ["negge", "neglt", "triincl", "ustrict", "mstrict", "mincl",
                                "ident_f", "ones_b", "ones_f"])
        K = C["_key"]
        scale = DH ** -0.5

        QT = [cx.sb([128, L], BF16, "QT%d" % i) for i in range(2)]
        KT = [cx.sb([128, L], BF16, "KT%d" % i) for i in range(2)]
        V = [cx.sb([128, NBLK, 128], BF16, "V%d" % i) for i in range(2)]
        FL = cx.sb([128, NBLK], F32, "FL")
        negb = cx.sb([128, 1], F32, "negb")
        banks = [cx.ps([128, 512], F32, "bank%d" % i) for i in range(8)]
        bk = ["bank%d" % i for i in range(8)]
        st1 = contextlib.ExitStack()
        cx1 = Ctx(nc, st1)
        W = cx1.sb([128, 16, 772], BF16, "W")
        hTt = [cx1.sb([128, 16, 512], BF16, "hTt%d" % i) for i in range(2)]

        wv = wA.rearrange("(c p) n -> p c n", p=128)
        for c4 in range(4):
            P.dma(lambda e, c4=c4: e.dma_start(out=W[:, 4 * c4:4 * c4 + 4, 0:769], in_=wv[:, 4 * c4:4 * c4 + 4, :]),
                  writes=[("W", c4)], q="pool")
        P.dma(lambda e: e.dma_start(out=negb[:], in_=bfg.partition_broadcast(128)), writes=["negb"], q="sp")
        P.dve(lambda e: e.tensor_scalar(out=negb[:], in0=negb[:], scalar1=-1.0, scalar2=None, op0=ALU.mult),
              reads=["negb"], writes=["negb"])
        Wk = [("W", i) for i in range(4)]

        tiles = [(0, 1)] + [(1 + 4 * i, 4) for i in range(16)]
        hv = hT.rearrange("(c p) t -> p c t", p=128)

        rr = 0
        import os
        _nt = int(os.environ.get("A_NT", "17"))
        _dov = int(os.environ.get("A_DOV", "1"))
        _doq = int(os.environ.get("A_DOQ", "1"))
        for ti, (qb0, nb) in enumerate(tiles):
            if ti >= _nt:
                break
            t0 = qb0 * 128
            n = nb * 128
            hb = hTt[ti % 2]
            hk = "hTt%d" % (ti % 2)
            for c4 in range(4):
                P.dma(lambda e, hb=hb, t0=t0, n=n, c4=c4: e.dma_start(
                    out=hb[:, 4 * c4:4 * c4 + 4, 0:n], in_=hv[:, 4 * c4:4 * c4 + 4, t0:t0 + n]),
                    writes=[(hk, c4)], q="sp")
            for which, (dst, col, sc) in enumerate([(QT[0], 0, scale), (KT[0], 128, 1.0),
                                                    (QT[1], 256, scale), (KT[1], 384, 1.0)][:4 * _doq]):
                b = rr % 8
                rr += 1
                for c in range(16):
                    P.pe(lambda e, b=b, c=c, col=col, hb=hb, n=n: e.matmul(
                        banks[b][:, 0:n], W[:, c, col:col + 128], hb[:, c, 0:n],
                        start=(c == 0), stop=(c == 15)),
                        reads=Wk + [(hk, c // 4)], writes=[bk[b]])
                dk = ("QK", which, ti)
                if which % 2 == 0:
                    P.act(lambda e, b=b, dst=dst, t0=t0, n=n, sc=sc: e.activation(
                        out=dst[:, t0:t0 + n], in_=banks[b][:, 0:n], func=AF.Copy, scale=sc),
                        reads=[bk[b]], writes=[dk])
                else:
                    P.dve(lambda e, b=b, dst=dst, t0=t0, n=n: e.tensor_copy(out=dst[:, t0:t0 + n], in_=banks[b][:, 0:n]),
                          reads=[bk[b]], writes=[dk])
            for j in range(nb * _dov):
                blk = qb0 + j
                b = rr % 8
                rr += 1
                for c in range(16):
                    P.pe(lambda e, b=b, c=c, hb=hb, j=j: e.matmul(
                        banks[b][:, 0:257], hb[:, c, j * 128:(j + 1) * 128], W[:, c, 512:769],
                        start=(c == 0), stop=(c == 15)),
                        reads=Wk + [(hk, c // 4)], writes=[bk[b]])
                P.act(lambda e, b=b, blk=blk: e.activation(out=V[0][:, blk, :], in_=banks[b][:, 0:128], func=AF.Copy),
                      reads=[bk[b]], writes=[("V", 0, blk)])
                P.dve(lambda e, b=b, blk=blk: e.tensor_copy(out=V[1][:, blk, :], in_=banks[b][:, 128:256]),
                      reads=[bk[b]], writes=[("V", 1, blk)])
                P.dve(lambda e, b=b, blk=blk: e.tensor_copy(out=FL[:, blk:blk + 1], in_=banks[b][:, 256:257]),
                      reads=[bk[b]], writes=["FL"])

        if upto < 2:
            P.emit()
            st1.close()
            return nc
        P.barrier()
        st1.close()
        spf = cx.sb([128, NBLK], F32, "spf")
        P.act(lambda e: e.activation(out=spf[:], in_=FL[:], func=AF.Exp, bias=negb[:, 0:1], scale=-1.0),
              reads=["FL", "negb"], writes=["spf"])
        P.act(lambda e: e.activation(out=spf[:], in_=spf[:], func=AF.Ln, bias=1.0, scale=1.0),
              reads=["spf"], writes=["spf"])
        P.dve(lambda e: e.memset(spf[0:NPAD, 0:1], 0.0), reads=["spf"], writes=["spf"])
        P.pe(lambda e: e.matmul(banks[0][0:NBLK, 0:128], spf[:, :], C["ones_f"][:, :], start=True, stop=True),
             reads=["spf", K["ones_f"]], writes=[bk[0]])
        abc = cx.sb([NBLK, 128], F32, "abc")
        P.dve(lambda e: e.tensor_copy(out=abc[:], in_=banks[0][0:NBLK, 0:128]), reads=[bk[0]], writes=["abc"])
        P.pe(lambda e: e.matmul(banks[1][0:NBLK, 0:128], C["ustrict"][0:NBLK, 0:NBLK], abc[:, :],
                                start=True, stop=False), reads=["abc", K["ustrict"]], writes=[bk[1]])
        P.pe(lambda e: e.matmul(banks[1][0:NBLK, 0:128], spf[:, :], C["triincl"][:, :],
                                start=False, stop=True), reads=["spf", K["triincl"]], writes=[bk[1]])
        cp = cx.sb([NBLK, 128], F32, "cp")
        P.dve(lambda e: e.tensor_copy(out=cp[:], in_=banks[1][0:NBLK, 0:128]), reads=[bk[1]], writes=["cp"])
        parts_p = cx.sb([NBLK, 3, 128], BF16, "parts_p")
        parts_n = cx.sb([NBLK, 3, 128], BF16, "parts_n")
        tmpf = cx.sb([NBLK, 128], F32, "tmpf")
        for k in range(3):
            P.dve(lambda e, k=k: e.tensor_copy(out=parts_p[:, k, :], in_=cp[:]), reads=["cp"], writes=["parts_p"])
            P.dve(lambda e, k=k: e.tensor_copy(out=tmpf[:], in_=parts_p[:, k, :]), reads=["parts_p"], writes=["tmpf"])
            P.dve(lambda e, k=k: e.tensor_sub(out=cp[:], in0=cp[:], in1=tmpf[:]), reads=["cp", "tmpf"], writes=["cp"])
        P.dve(lambda e: e.tensor_scalar(out=parts_n[:], in0=parts_p[:], scalar1=-1.0, scalar2=None, op0=ALU.mult),
              reads=["parts_p"], writes=["parts_n"])
        P.dve(lambda e: e.memset(parts_p[0:1, 0, 0:NPAD], -30000.0), reads=["parts_p", "parts_n"], writes=["parts_p"])
        LK = cx.sb([6, L], BF16, "LK")
        RQ = cx.sb([6, L], BF16, "RQ")
        P.pool(lambda e: e.memset(LK[:], 1.0), writes=["LK"])
        P.pool(lambda e: e.memset(RQ[:], 1.0), writes=["RQ"])
        P.dma(lambda e: e.dma_start(out=cscr[0:3, :].rearrange("k (b p) -> b k p", p=128), in_=parts_p[:]),
              reads=["parts_p"], writes=["cscr_p"], q="sp")
        P.dma(lambda e: e.dma_start(out=cscr[3:6, :].rearrange("k (b p) -> b k p", p=128), in_=parts_n[:]),
              reads=["parts_n"], writes=["cscr_n"], q="sp")
        P.dma(lambda e: e.dma_start(out=LK[3:6, :], in_=cscr[0:3, :]), reads=["cscr_p", "LK"], writes=["LK"], q="sp")
        P.dma(lambda e: e.dma_start(out=RQ[0:3, :], in_=cscr[3:6, :]), reads=["cscr_n", "RQ"], writes=["RQ"], q="sp")

        if upto < 3:
            P.emit()
            return nc
        e_t = [cx.sb([128, 512], F32, "e%d" % i) for i in range(2)]
        sp_t = [cx.sb([128, 512], BF16, "sp%d" % i) for i in range(3)]
        g_t = [cx.sb([128, 512], F32, "g%d" % i) for i in range(2)]
        w_t = [cx.sb([128, 512], BF16, "w%d" % i) for i in range(2)]
        p_t = [cx.sb([128, 512], BF16, "pp%d" % i) for i in range(2)]
        yst = [cx.sb([128, 512], BF16, "yst%d" % i) for i in range(4)]
        rec = cx.sb([128, 512], F32, "rec")
        ZB = [0, 1]
        AB, OB = 2, 3
        SB_ = [4, 5]
        O2, DB = 6, 7

        def do_tile(ti, qb0, nb):
            t0 = qb0 * 128
            n = nb * 128
            nsteps = qb0 + nb

            def kkeys(jb):
                tj = 0 if jb == 0 else 1 + (jb - 1) // 4
                return tj

            P.dve(lambda e, n=n: e.memset(banks[AB][:, 0:n], 0.0), writes=[bk[AB]])
            P.dve(lambda e, n=n: e.memset(banks[OB][:, 0:n], 0.0), writes=[bk[OB]])
            P.dve(lambda e, n=n: e.memset(banks[O2][:, 0:n], 0.0), writes=[bk[O2]])
            P.dve(lambda e, n=n: e.memset(banks[DB][:, 0:n], 0.0), writes=[bk[DB]])

            def cols(i):
                jb = qb0 + nb - 1 - i
                r = jb - qb0
                c0 = max(r, 0) * 128
                return jb, r, c0

            def sbA(i):
                jb, r, c0 = cols(i)
                zb = ZB[i % 2]
                eb = e_t[i % 2]
                ek = "e%d" % (i % 2)
                spb = sp_t[i % 3]
                spk = "sp%d" % (i % 3)
                tj = kkeys(jb)
                P.pe(lambda e: e.matmul(banks[zb][:, c0:n], KT[0][:, jb * 128:(jb + 1) * 128], QT[0][:, t0 + c0:t0 + n],
                                        start=True, stop=True),
                     reads=[("QK", 1, tj), ("QK", 0, ti)], writes=[bk[zb]])
                P.act(lambda e: e.activation(out=eb[:, c0:n], in_=banks[zb][:, c0:n], func=AF.Exp),
                      reads=[bk[zb]], writes=[ek])
                if r >= 0:
                    P.dve(lambda e: e.tensor_mul(out=eb[:, c0:c0 + 128], in0=eb[:, c0:c0 + 128], in1=C["mstrict"][:]),
                          reads=[ek, K["mstrict"]], writes=[ek])
                if jb == 0:
                    P.dve(lambda e: e.memset(eb[0:NPAD, c0:n], 0.0), reads=[ek], writes=[ek])

            def sbA2(i):
                jb, r, c0 = cols(i)
                eb = e_t[i % 2]
                ek = "e%d" % (i % 2)
                spb = sp_t[i % 3]
                spk = "sp%d" % (i % 3)
                P.act(lambda e: e.activation(out=spb[:, c0:n], in_=eb[:, c0:n], func=AF.Ln, bias=1.0, scale=1.0),
                      reads=[ek], writes=[spk])

            def sbB(i):
                jb, r, c0 = cols(i)
                spb = sp_t[i % 3]
                spk = "sp%d" % (i % 3)
                eb = e_t[i % 2]
                ek = "e%d" % (i % 2)
                gb = g_t[i % 2]
                gk = "g%d" % (i % 2)
                wb = w_t[i % 2]
                wk = "w%d" % (i % 2)
                if i > 0:
                    _, _, pc0 = cols(i - 1)
                    spp = sp_t[(i - 1) % 3]
                    sppk = "sp%d" % ((i - 1) % 3)
                    P.pe(lambda e: e.matmul(banks[AB][:, pc0:n], C["neglt"][:], spp[:, pc0:n],
                                            start=False, stop=True, skip_group_check=True),
                         reads=[sppk, K["neglt"]], writes=[bk[AB]])
                P.pe(lambda e: e.matmul(banks[AB][:, c0:n], C["negge"][:], spb[:, c0:n],
                                        start=False, stop=True, skip_group_check=True),
                     reads=[spk, K["negge"]], writes=[bk[AB]])
                P.act(lambda e: e.activation(out=gb[:, c0:n], in_=banks[AB][:, c0:n], func=AF.Exp),
                      reads=[bk[AB]], writes=[gk])
                P.dve(lambda e: e.tensor_mul(out=wb[:, c0:n], in0=eb[:, c0:n], in1=gb[:, c0:n]),
                      reads=[ek, gk], writes=[wk])

            def sbC(i):
                jb, r, c0 = cols(i)
                wb = w_t[i % 2]
                wk = "w%d" % (i % 2)
                P.pe(lambda e: e.matmul(banks[OB][:, c0:n], V[0][:, jb, :], wb[:, c0:n],
                                        start=False, stop=True, skip_group_check=True),
                     reads=[wk, ("V", 0, jb)], writes=[bk[OB]])

            def fxA(i):
                jb, r, c0 = cols(i)
                sb = SB_[i % 2]
                pb = p_t[i % 2]
                pk = "pp%d" % (i % 2)
                tj = kkeys(jb)
                P.pe(lambda e: e.matmul(banks[sb][:, c0:n], KT[1][:, jb * 128:(jb + 1) * 128], QT[1][:, t0 + c0:t0 + n],
                                        start=True, stop=False),
                     reads=[("QK", 3, tj), ("QK", 2, ti)], writes=[bk[sb]])
                P.pe(lambda e: e.matmul(banks[sb][:, c0:n], LK[0:6, jb * 128:(jb + 1) * 128], RQ[0:6, t0 + c0:t0 + n],
                                        start=False, stop=True),
                     reads=["LK", "RQ"], writes=[bk[sb]])
                P.act(lambda e: e.activation(out=pb[:, c0:n], in_=banks[sb][:, c0:n], func=AF.Exp),
                      reads=[bk[sb]], writes=[pk])
                if r >= 0:
                    P.dve(lambda e: e.tensor_mul(out=pb[:, c0:c0 + 128], in0=pb[:, c0:c0 + 128], in1=C["mincl"][:]),
                          reads=[pk, K["mincl"]], writes=[pk])

            def fxB(i):
                jb, r, c0 = cols(i)
                pb = p_t[i % 2]
                pk = "pp%d" % (i % 2)
                P.pe(lambda e: e.matmul(banks[O2][:, c0:n], V[1][:, jb, :], pb[:, c0:n],
                                        start=False, stop=True, skip_group_check=True),
                     reads=[pk, ("V", 1, jb)], writes=[bk[O2]])
                P.pe(lambda e: e.matmul(banks[DB][:, c0:n], C["ones_b"][:], pb[:, c0:n],
                                        start=False, stop=True, skip_group_check=True),
                     reads=[pk, K["ones_b"]], writes=[bk[DB]])

            for it in range(nsteps + 2):
                if it < nsteps:
                    sbA(it)
                    fxA(it)
                    sbA2(it)
                if 0 <= it - 1 < nsteps:
                    sbB(it - 1)
                    fxB(it - 1)
                if 0 <= it - 2 < nsteps:
                    sbC(it - 2)

            ys = yst[(2 * ti) % 4]
            ysk = "yst%d" % ((2 * ti) % 4)
            yf = yst[(2 * ti + 1) % 4]
            yfk = "yst%d" % ((2 * ti + 1) % 4)
            P.act(lambda e, ys=ys, n=n: e.activation(out=ys[:, 0:n], in_=banks[OB][:, 0:n], func=AF.Copy),
                  reads=[bk[OB]], writes=[ysk])
            P.dma(lambda e, ys=ys, n=n, t0=t0: e.dma_start(out=yT[0:128, t0:t0 + n], in_=ys[:, 0:n]),
                  reads=[ysk], writes=[("yT", 0, ti)], q="sp")
            P.dve(lambda e, n=n: e.tensor_scalar(out=rec[:, 0:n], in0=banks[DB][:, 0:n], scalar1=1e-30, scalar2=None,
                                                 op0=ALU.add), reads=[bk[DB]], writes=["rec"])
            P.dve(lambda e, n=n: e.reciprocal(out=rec[:, 0:n], in_=rec[:, 0:n]), reads=["rec"], writes=["rec"])
            P.dve(lambda e, yf=yf, n=n: e.tensor_mul(out=yf[:, 0:n], in0=banks[O2][:, 0:n], in1=rec[:, 0:n]),
                  reads=[bk[O2], "rec"], writes=[yfk])
            P.dma(lambda e, yf=yf, n=n, t0=t0: e.dma_start(out=yT[128:256, t0:t0 + n], in_=yf[:, 0:n]),
                  reads=[yfk], writes=[("yT", 1, ti)], q="sp")

        for ti, (qb0, nb) in enumerate(tiles):
            do_tile(ti, qb0, nb)
        P.emit()
    return nc


def build_T(first):
    nc = bass.Bass("TRN2", target_bir_lowering=False)
    io = {}
    def din(name, shape, dt=F32):
        io[name] = nc.dram_tensor(name, list(shape), dt, kind="ExternalInput").ap()
    if first:
        din("xin", [NT, D]); din("lng", [2, D])
    else:
        din("hprev", [NT, D]); din("hTp", [D, NT], BF16); din("yT", [D, NT], BF16)
        din("wg", [D, 2 * D]); din("wbs", [1024, D]); din("wbf", [1024, D]); din("wo", [D, D])
        din("lnm", [2, D]); din("wr", [D, NE]); din("rb", [1, NE])
        import os
        _ne = int(os.environ.get("T_NEXP", NE))
        din("wgate", [_ne, D, FF]); din("wup", [_ne, D, FF]); din("wdown", [_ne, FF, D]); din("lnf", [2, D])
    io["h"] = nc.dram_tensor("h", [NT, D], F32, kind="ExternalOutput").ap()
    io["hT"] = nc.dram_tensor("hT", [D, NT], BF16, kind="ExternalOutput").ap()
    emit_T(nc, io, first)
    return nc


def emit_T(nc, io, first):
    import contextlib
    with contextlib.ExitStack() as st:
        cx = Ctx(nc, st)
        P = Prog(nc)
        C = make_consts(P, cx, ["ident_f", "ident_b", "ustrict_b", "ones_b"])
        K = C["_key"]
        fb = [cx.ps([128, 512], F32, "bankf%d" % i) for i in range(6)]
        fk = ["bankf%d" % i for i in range(6)]
        tb = [cx.ps([128, 1024], BF16, "bankt%d" % i) for i in range(2)]
        tk = ["bankt%d" % i for i in range(2)]
        rr = [0]

        def nbank():
            rr[0] += 1
            return rr[0] % 6

        X = cx.sb([128, TB, D], F32, "X")
        XK = lambda b: ("X", b)
        st_s = cx.sb([128, 8], F32, "st_s")

        lncnt = [0]

        def layer_norm(gb_ap, cxl):
            lncnt[0] += 1
            G = cxl.sb([128, D], F32, "lnG%d" % lncnt[0])
            Bt = cxl.sb([128, D], F32, "lnB%d" % lncnt[0])
            junk = cxl.sb([128, D], BF16, "lnjunk%d" % lncnt[0])
            P.dma(lambda e: e.dma_start(out=G[:], in_=gb_ap[0:1, :].partition_broadcast(128)), writes=["lnG"])
            P.dma(lambda e: e.dma_start(out=Bt[:], in_=gb_ap[1:2, :].partition_broadcast(128)), writes=["lnB"])
            for b in range(TB):
                xs = X[:, b, :]
                P.dve(lambda e, xs=xs: e.reduce_sum(out=st_s[:, 0:1], in_=xs, axis=AX.X), reads=[XK(b)], writes=["st_s"])
                P.dve(lambda e: e.memset(st_s[:, 1:2], 0.0), writes=["st_s1"])
                P.act(lambda e, xs=xs: e.activation(out=junk[:], in_=xs, func=AF.Square, accum_out=st_s[:, 1:2]),
                      reads=[XK(b), "st_s1"], writes=["st_s1", "lnjunk"])
                P.dve(lambda e: e.tensor_scalar(out=st_s[:, 2:3], in0=st_s[:, 0:1], scalar1=1.0 / D, scalar2=None,
                                                op0=ALU.mult), reads=["st_s"], writes=["st_s2"])
                P.dve(lambda e: e.tensor_mul(out=st_s[:, 3:4], in0=st_s[:, 2:3], in1=st_s[:, 2:3]),
                      reads=["st_s2"], writes=["st_s3"])
                P.dve(lambda e: e.scalar_tensor_tensor(out=st_s[:, 4:5], in0=st_s[:, 1:2], scalar=1.0 / D,
                                                       in1=st_s[:, 3:4], op0=ALU.mult, op1=ALU.subtract),
                      reads=["st_s1", "st_s3"], writes=["st_s4"])
                P.act(lambda e: e.activation(out=st_s[:, 5:6], in_=st_s[:, 4:5], func=AF.Sqrt, bias=EPS_T[:, 0:1], scale=1.0),
                      reads=["st_s4", "eps"], writes=["st_s5"])
                P.dve(lambda e: e.reciprocal(out=st_s[:, 6:7], in_=st_s[:, 5:6]), reads=["st_s5"], writes=["st_s6"])
                P.dve(lambda e, xs=xs: e.tensor_scalar(out=xs, in0=xs, scalar1=st_s[:, 2:3], scalar2=st_s[:, 6:7],
                                                       op0=ALU.subtract, op1=ALU.mult),
                      reads=[XK(b), "st_s2", "st_s6"], writes=[XK(b)])
                P.dve(lambda e, xs=xs: e.tensor_mul(out=xs, in0=xs, in1=G[:]), reads=[XK(b), "lnG"], writes=[XK(b)])
                P.pool(lambda e, xs=xs: e.tensor_add(out=xs, in0=xs, in1=Bt[:]), reads=[XK(b), "lnB"], writes=[XK(b)])

        EPS_T = cx.sb([128, 1], F32, "eps")
        P.pool(lambda e: e.memset(EPS_T[:], EPS), writes=["eps"])

        def write_outputs(Hb, cxl):
            hv = io["h"].rearrange("(b p) d -> p b d", p=128)
            hTv = io["hT"].rearrange("(c p) t -> p c t", p=128)
            hts = [cxl.sb([128, 16, 128], BF16, "hts%d" % i) for i in range(2)]
            for b in range(TB):
                P.dma(lambda e, b=b: e.dma_start(out=hv[:, b, :], in_=X[:, b, :]), reads=[XK(b)], writes=[("hout", b)])
                P.act(lambda e, b=b: e.activation(out=Hb[:, b, :], in_=X[:, b, :], func=AF.Copy),
                      reads=[XK(b)], writes=[("Hb", b)])
                hs = hts[b % 2]
                hk = "hts%d" % (b % 2)
                for half in range(2):
                    t = tb[half]
                    for c in range(8):
                        cc = half * 8 + c
                        P.pe(lambda e, t=t, c=c, cc=cc, b=b: e.transpose(t[:, c * 128:(c + 1) * 128],
                                                                       Hb[:, b, cc * 128:(cc + 1) * 128], C["ident_b"][:]),
                             reads=[("Hb", b), K["ident_b"]], writes=[tk[half]])
                    if half == 0:
                        P.dve(lambda e, t=t, hs=hs: e.tensor_copy(out=hs[:, 0:8, :], in_=t[:].rearrange("p (c t) -> p c t", t=128)),
                              reads=[tk[half]], writes=[(hk, 0)])
                    else:
                        P.act(lambda e, t=t, hs=hs: e.activation(out=hs[:, 8:16, :], in_=t[:].rearrange("p (c t) -> p c t", t=128),
                                                                func=AF.Copy), reads=[tk[half]], writes=[(hk, 1)])
                P.dma(lambda e, b=b, hs=hs: e.dma_start(out=hTv[:, :, b * 128:(b + 1) * 128], in_=hs[:]),
                      reads=[(hk, 0), (hk, 1)], writes=[("hTout", b)])

        if first:
            xv = io["xin"].rearrange("(b p) d -> p b d", p=128)
            for b in range(TB):
                P.dma(lambda e, b=b: e.dma_start(out=X[:, b, :], in_=xv[:, b, :]), writes=[XK(b)])
            Hb = cx.sb([128, TB, D], BF16, "Hb")
            layer_norm(io["lng"], cx)
            write_outputs(Hb, cx)
            P.emit()
            return

        stA = contextlib.ExitStack()
        cxA = Ctx(nc, stA)
        MT = cxA.sb([128, 16, NT], BF16, "MT")
        stA1 = contextlib.ExitStack()
        cxA1 = Ctx(nc, stA1)
        Xb = X[:].rearrange("p b d -> p (b d)").bitcast(BF16)
        hTp = Xb[:, 0:16 * NT].rearrange("p (c t) -> p c t", t=NT)
        yTs = Xb[:, 16 * NT:32 * NT].rearrange("p (c t) -> p c t", t=NT)
        hv_ = io["hTp"].rearrange("(c p) t -> p c t", p=128)
        yv_ = io["yT"].rearrange("(c p) t -> p c t", p=128)
        for c4 in range(4):
            P.dma(lambda e, c4=c4: e.dma_start(out=hTp[:, 4 * c4:4 * c4 + 4, :], in_=hv_[:, 4 * c4:4 * c4 + 4, :]),
                  writes=[("hTp", c4)])
            P.dma(lambda e, c4=c4: e.dma_start(out=yTs[:, 4 * c4:4 * c4 + 4, :], in_=yv_[:, 4 * c4:4 * c4 + 4, :]),
                  writes=[("yTs", c4)])
        wgs = [cxA1.sb([128, 16, 128], BF16, "wgs%d" % i) for i in range(2)]
        wgf = [cxA1.sb([128, 16, 128], BF16, "wgf%d" % i) for i in range(2)]
        wbs = [cxA1.sb([128, 8, 128], BF16, "wbs%d" % i) for i in range(2)]
        wbf = [cxA1.sb([128, 8, 128], BF16, "wbf%d" % i) for i in range(2)]
        sgs = [cxA1.sb([128, 384], F32, "sgs%d" % i) for i in range(2)]
        sgf = [cxA1.sb([128, 384], F32, "sgf%d" % i) for i in range(2)]
        m1 = [cxA1.sb([128, 384], F32, "m1_%d" % i) for i in range(2)]
        m2 = [cxA1.sb([128, 384], F32, "m2_%d" % i) for i in range(2)]
        wgv = io["wg"].rearrange("(c p) n -> p c n", p=128)
        wbsv = io["wbs"].rearrange("(c p) n -> p c n", p=128)
        wbfv = io["wbf"].rearrange("(c p) n -> p c n", p=128)
        it = 0
        for nci in range(16):
            w = nci % 2
            n0 = nci * 128
            for hf in range(2):
                P.dma(lambda e, w=w, n0=n0, hf=hf: e.dma_start(out=wgs[w][:, 8 * hf:8 * hf + 8, :],
                                                              in_=wgv[:, 8 * hf:8 * hf + 8, n0:n0 + 128]),
                      writes=[("wgs", w, hf)], q="pool")
                P.dma(lambda e, w=w, n0=n0, hf=hf: e.dma_start(out=wgf[w][:, 8 * hf:8 * hf + 8, :],
                                                              in_=wgv[:, 8 * hf:8 * hf + 8, D + n0:D + n0 + 128]),
                      writes=[("wgf", w, hf)], q="pool")
            P.dma(lambda e, w=w, n0=n0: e.dma_start(out=wbs[w][:], in_=wbsv[:, :, n0:n0 + 128]), writes=[("wbs", w)], q="pool")
            P.dma(lambda e, w=w, n0=n0: e.dma_start(out=wbf[w][:], in_=wbfv[:, :, n0:n0 + 128]), writes=[("wbf", w)], q="pool")
            for tr in range(3):
                ts = slice(tr * 384, (tr + 1) * 384)
                u = it % 2
                it += 1
                bg, bb_, bg2, bb2 = nbank(), nbank(), nbank(), nbank()
                for c in range(16):
                    P.pe(lambda e, c=c, w=w, ts=ts, bg=bg: e.matmul(fb[bg][:, 0:384], wgs[w][:, c, :], hTp[:, c, ts],
                                                                    start=(c == 0), stop=(c == 15)),
                         reads=[("wgs", w, c // 8), ("hTp", c // 4)], writes=[fk[bg]])
                for c in range(8):
                    P.pe(lambda e, c=c, w=w, ts=ts, bb_=bb_: e.matmul(fb[bb_][:, 0:384], wbs[w][:, c, :], yTs[:, c, ts],
                                                                      start=(c == 0), stop=(c == 7)),
                         reads=[("wbs", w), ("yTs", c // 4)], writes=[fk[bb_]])
                for c in range(16):
                    P.pe(lambda e, c=c, w=w, ts=ts, bg2=bg2: e.matmul(fb[bg2][:, 0:384], wgf[w][:, c, :], hTp[:, c, ts],
                                                                      start=(c == 0), stop=(c == 15)),
                         reads=[("wgf", w, c // 8), ("hTp", c // 4)], writes=[fk[bg2]])
                for c in range(8):
                    P.pe(lambda e, c=c, w=w, ts=ts, bb2=bb2: e.matmul(fb[bb2][:, 0:384], wbf[w][:, c, :], yTs[:, 8 + c, ts],
                                                                      start=(c == 0), stop=(c == 7)),
                         reads=[("wbf", w), ("yTs", 2 + c // 4)], writes=[fk[bb2]])
                P.act(lambda e, u=u, bg=bg: e.activation(out=sgs[u][:], in_=fb[bg][:, 0:384], func=AF.Sigmoid),
                      reads=[fk[bg]], writes=[("sgs", u)])
                P.act(lambda e, u=u, bg2=bg2: e.activation(out=sgf[u][:], in_=fb[bg2][:, 0:384], func=AF.Sigmoid),
                      reads=[fk[bg2]], writes=[("sgf", u)])
                P.dve(lambda e, u=u, bb_=bb_: e.tensor_mul(out=m1[u][:], in0=sgs[u][:], in1=fb[bb_][:, 0:384]),
                      reads=[("sgs", u), fk[bb_]], writes=[("m1", u)])
                P.dve(lambda e, u=u, bb2=bb2: e.tensor_mul(out=m2[u][:], in0=sgf[u][:], in1=fb[bb2][:, 0:384]),
                      reads=[("sgf", u), fk[bb2]], writes=[("m2", u)])
                P.pool(lambda e, u=u, nci=nci, ts=ts: e.tensor_add(out=MT[:, nci, ts], in0=m1[u][:], in1=m2[u][:]),
                       reads=[("m1", u), ("m2", u)], writes=[("MT", nci)])
        import os
        _stop = int(os.environ.get("T_STOP", "9"))
        if _stop <= 1:
            P.emit(); stA1.close(); stA.close(); return
        P.barrier()
        stA1.close()

        stA2 = contextlib.ExitStack()
        cxA2 = Ctx(nc, stA2)
        hpv = io["hprev"].rearrange("(b p) d -> p b d", p=128)
        for b in range(TB):
            P.dma(lambda e, b=b: e.dma_start(out=X[:, b, :], in_=hpv[:, b, :]), writes=[XK(b)])
        woc = [cxA2.sb([128, 16, 512], BF16, "woc%d" % i) for i in range(2)]
        wov = io["wo"].rearrange("(c p) n -> p c n", p=128)
        for cg in range(4):
            w = cg % 2
            for q4 in range(4):
                P.dma(lambda e, w=w, cg=cg, q4=q4: e.dma_start(out=woc[w][:, 4 * q4:4 * q4 + 4, :],
                                                              in_=wov[:, 4 * q4:4 * q4 + 4, cg * 512:(cg + 1) * 512]),
                      writes=[("woc", w, q4)], q="pool")
            for b in range(TB):
                bo = nbank()
                for c in range(16):
                    P.pe(lambda e, c=c, w=w, b=b, bo=bo: e.matmul(fb[bo][:, :], MT[:, c, b * 128:(b + 1) * 128], woc[w][:, c, :],
                                                                  start=(c == 0), stop=(c == 15)),
                         reads=[("MT", c), ("woc", w, c // 4)], writes=[fk[bo]])
                P.dve(lambda e, b=b, cg=cg, bo=bo: e.scalar_tensor_tensor(
                    out=X[:, b, cg * 512:(cg + 1) * 512], in0=X[:, b, cg * 512:(cg + 1) * 512], scalar=ALPHA,
                    in1=fb[bo][:, :], op0=ALU.mult, op1=ALU.add), reads=[XK(b), fk[bo]], writes=[XK(b)])
        layer_norm(io["lnm"], cxA2)
        if _stop <= 2:
            P.emit(); stA2.close(); stA.close(); return
        P.barrier()
        stA2.close()
        stA.close()

        Hb = cx.sb([128, TB, D], BF16, "Hb")
        RT = cx.sb([128, TB, NE], F32, "aff")
        MSK = cx.sb([128, TB, NE], F32, "msk")
        WN = cx.sb([128, TB, NE], F32, "wn")
        POS = cx.sb([128, TB, NE], F32, "pos")
        WHL = cx.sb([128, TB, NE, 2], BF16, "whl")
        iota_f = cx.sb([128, CAP], F32, "iota_f")
        iota_i = cx.sb([128, CAP], I32, "iota_i")
        P.pool(lambda e: e.iota(iota_i[:], pattern=[[1, CAP]], base=0, channel_multiplier=0), writes=["iota_i"])
        P.pool(lambda e: e.tensor_copy(out=iota_f[:], in_=iota_i[:]), reads=["iota_i"], writes=["iota_f"])
        stC = contextlib.ExitStack()
        cxC = Ctx(nc, stC)
        wr_sb = cxC.sb([128, 16, NE], F32, "wr_sb")
        rb_sb = cxC.sb([128, NE], F32, "rb_sb")
        wrv = io["wr"].rearrange("(c p) e -> p c e", p=128)
        for c4 in range(4):
            P.dma(lambda e, c4=c4: e.dma_start(out=wr_sb[:, 4 * c4:4 * c4 + 4, :], in_=wrv[:, 4 * c4:4 * c4 + 4, :]),
                  writes=[("wr", c4)])
        P.dma(lambda e: e.dma_start(out=rb_sb[:], in_=io["rb"].partition_broadcast(128)), writes=["rb"])
        xT32 = [cxC.sb([128, 16, 128], F32, "xT32_%d" % i) for i in range(2)]
        for b in range(TB):
            P.act(lambda e, b=b: e.activation(out=Hb[:, b, :], in_=X[:, b, :], func=AF.Copy), reads=[XK(b)], writes=[("Hb", b)])
            xt = xT32[b % 2]
            xk = "xT32_%d" % (b % 2)
            for q4 in range(4):
                bt_ = nbank()
                for c in range(4):
                    cc = q4 * 4 + c
                    P.pe(lambda e, b=b, c=c, cc=cc, bt_=bt_: e.transpose(fb[bt_][:, c * 128:(c + 1) * 128],
                                                                        X[:, b, cc * 128:(cc + 1) * 128], C["ident_f"][:]),
                         reads=[XK(b), K["ident_f"]], writes=[fk[bt_]])
                if q4 % 2 == 0:
                    P.dve(lambda e, xt=xt, q4=q4, bt_=bt_: e.tensor_copy(out=xt[:, 4 * q4:4 * q4 + 4, :],
                                                                        in_=fb[bt_][:].rearrange("p (c t) -> p c t", t=128)),
                          reads=[fk[bt_]], writes=[(xk, q4)])
                else:
                    P.act(lambda e, xt=xt, q4=q4, bt_=bt_: e.activation(out=xt[:, 4 * q4:4 * q4 + 4, :],
                                                                       in_=fb[bt_][:].rearrange("p (c t) -> p c t", t=128),
                                                                       func=AF.Copy), reads=[fk[bt_]], writes=[(xk, q4)])
            br = nbank()
            for c in range(16):
                P.pe(lambda e, c=c, xt=xt, br=br: e.matmul(fb[br][:, 0:NE], xt[:, c, :], wr_sb[:, c, :],
                                                           start=(c == 0), stop=(c == 15)),
                     reads=[(xk, c // 4), ("wr", c // 4)], writes=[fk[br]])
            P.act(lambda e, b=b, br=br: e.activation(out=RT[:, b, :], in_=fb[br][:, 0:NE], func=AF.Sigmoid),
                  reads=[fk[br]], writes=[("aff", b)])
        for b in range(TB):
            P.pool(lambda e, b=b: e.tensor_scalar(out=X[:, b, :], in0=X[:, b, :], scalar1=ALPHA, scalar2=None, op0=ALU.mult),
                   reads=[XK(b)], writes=[XK(b)])
        affk = [("aff", b) for b in range(TB)]
        sel = cxC.sb([128, TB, NE], F32, "sel")
        sel2 = cxC.sb([128, TB, NE], F32, "sel2")
        eq = cxC.sb([128, TB, NE], F32, "eq")
        mx1 = cxC.sb([128, TB * 4], F32, "mx1")
        mx2 = cxC.sb([128, TB * 4], F32, "mx2")
        gs = cxC.sb([128, TB, 4], F32, "gs")
        gmax = cxC.sb([128, TB], F32, "gmax")
        best = cxC.sb([128, TB * 4], F32, "best")
        wsum = cxC.sb([128, TB], F32, "wsum")
        tmpw = cxC.sb([128, TB, NE], F32, "tmpw")
        for b in range(TB):
            P.dve(lambda e, b=b: e.tensor_add(out=sel[:, b, :], in0=RT[:, b, :], in1=rb_sb[:]),
                  reads=[("aff", b), "rb"], writes=["sel"])
        s4 = lambda t: t[:].rearrange("p b (g k) -> p (b g) k", k=4)
        bc = lambda t: t[:].unsqueeze(2).to_broadcast([128, TB * 4, 4])
        P.dve(lambda e: e.tensor_reduce(out=mx1[:], in_=s4(sel), axis=AX.X, op=ALU.max), reads=["sel"], writes=["mx1"])
        P.dve(lambda e: e.tensor_tensor(out=s4(eq), in0=s4(sel), in1=bc(mx1), op=ALU.is_equal),
              reads=["sel", "mx1"], writes=["eq"])
        P.dve(lambda e: e.scalar_tensor_tensor(out=sel2[:], in0=eq[:], scalar=-1e9, in1=sel[:], op0=ALU.mult, op1=ALU.add),
              reads=["eq", "sel"], writes=["sel2"])
        P.dve(lambda e: e.tensor_reduce(out=mx2[:], in_=s4(sel2), axis=AX.X, op=ALU.max), reads=["sel2"], writes=["mx2"])
        P.dve(lambda e: e.tensor_add(out=gs[:].rearrange("p b g -> p (b g)"), in0=mx1[:], in1=mx2[:]),
              reads=["mx1", "mx2"], writes=["gs"])
        P.dve(lambda e: e.tensor_reduce(out=gmax[:], in_=gs[:], axis=AX.X, op=ALU.max), reads=["gs"], writes=["gmax"])
        P.dve(lambda e: e.tensor_tensor(out=best[:].rearrange("p (b g) -> p b g", g=4), in0=gs[:],
                                        in1=gmax[:].unsqueeze(2).to_broadcast([128, TB, 4]), op=ALU.is_equal),
              reads=["gs", "gmax"], writes=["best"])
        P.dve(lambda e: e.tensor_tensor(out=s4(eq), in0=s4(sel), in1=bc(mx2), op=ALU.is_ge),
              reads=["sel", "mx2", "sel2"], writes=["eq"])
        P.dve(lambda e: e.tensor_tensor(out=s4(MSK), in0=s4(eq), in1=bc(best), op=ALU.mult),
              reads=["eq", "best"], writes=["msk"])
        P.dve(lambda e: e.memset(MSK[0:NPAD, 0, :], 0.0), reads=["msk"], writes=["msk"])
        P.dve(lambda e: e.tensor_mul(out=tmpw[:], in0=MSK[:], in1=RT[:]), reads=["msk"] + affk, writes=["tmpw"])
        P.dve(lambda e: e.tensor_reduce(out=wsum[:], in_=tmpw[:], axis=AX.X, op=ALU.add), reads=["tmpw"], writes=["wsum"])
        P.dve(lambda e: e.tensor_scalar(out=wsum[:], in0=wsum[:], scalar1=1e-30, scalar2=None, op0=ALU.add),
              reads=["wsum"], writes=["wsum"])
        P.dve(lambda e: e.reciprocal(out=wsum[:], in_=wsum[:]), reads=["wsum"], writes=["wsum"])
        P.dve(lambda e: e.tensor_tensor(out=WN[:], in0=tmpw[:], in1=wsum[:].unsqueeze(2).to_broadcast([128, TB, NE]),
                                        op=ALU.mult), reads=["tmpw", "wsum"], writes=["wn"])
        P.dve(lambda e: e.tensor_copy(out=WHL[:, :, :, 0], in_=WN[:]), reads=["wn"], writes=["whl0"])
        P.dve(lambda e: e.tensor_copy(out=tmpw[:], in_=WHL[:, :, :, 0]), reads=["whl0", "wn"], writes=["tmpw"])
        P.dve(lambda e: e.tensor_sub(out=tmpw[:], in0=WN[:], in1=tmpw[:]), reads=["wn", "tmpw"], writes=["tmpw"])
        P.dve(lambda e: e.tensor_copy(out=WHL[:, :, :, 1], in_=tmpw[:]), reads=["tmpw"], writes=["whl1"])
        mskb = cxC.sb([128, TB, NE], BF16, "mskb")
        cmb = cxC.sb([128, TB, NE], BF16, "cmb")
        P.dve(lambda e: e.tensor_copy(out=mskb[:], in_=MSK[:]), reads=["msk"], writes=["mskb"])
        P.dve(lambda e: e.tensor_copy(out=cmb[:, 0, :], in_=mskb[:, 0, :]), reads=["mskb"], writes=[("cmb", 0)])
        for b in range(1, TB):
            P.dve(lambda e, b=b: e.tensor_add(out=cmb[:, b, :], in0=cmb[:, b - 1, :], in1=mskb[:, b, :]),
                  reads=["mskb", ("cmb", b - 1)], writes=[("cmb", b)])
        for b in range(TB):
            bp = nbank()
            P.pe(lambda e, b=b, bp=bp: e.matmul(fb[bp][:, 0:NE], C["ustrict_b"][:], mskb[:, b, :], start=True, stop=(b == 0)),
                 reads=["mskb", K["ustrict_b"]], writes=[fk[bp]])
            if b > 0:
                P.pe(lambda e, b=b, bp=bp: e.matmul(fb[bp][:, 0:NE], C["ones_b"][:], cmb[:, b - 1, :], start=False, stop=True),
                     reads=[("cmb", b - 1), K["ones_b"]], writes=[fk[bp]])
            P.dve(lambda e, b=b, bp=bp: e.tensor_copy(out=POS[:, b, :], in_=fb[bp][:, 0:NE]), reads=[fk[bp]], writes=["pos"])
        if _stop <= 3:
            P.emit(); stC.close(); return
        P.barrier()
        stC.close()

        stD = contextlib.ExitStack()
        cxD = Ctx(nc, stD)
        _S0 = cxD.sb([128, TB, CAP], BF16, "S0")
        S = [_S0, _S0]
        ST = [cxD.sb([128, 2, NT], BF16, "ST%d" % i) for i in range(2)]
        XeT2 = [cxD.sb([128, 16, CAP], BF16, "XeT%d" % i) for i in range(2)]
        AT = cxD.sb([128, 8, CAP], BF16, "AT")
        Yb = cxD.sb([128, 2, D], BF16, "Yb")
        wsl = [cxD.sb([128, 2], F32, "wsl%d" % i) for i in range(2)]
        sgl = [cxD.sb([128, CAP], F32, "sgl%d" % i) for i in range(2)]
        wgp = [cxD.sb([128, 16, 256], BF16, "wgp%d" % i) for i in range(2)]
        wup = [cxD.sb([128, 16, 256], BF16, "wup%d" % i) for i in range(2)]
        wdp = [cxD.sb([128, 8, 512], BF16, "wdp%d" % i) for i in range(2)]
        allX = [XK(b) for b in range(TB)]
        pcnt = [0]
        dcnt = [0]

        def part1(ex):
            s = ex % 2
            XeT = XeT2[s]
            Sk = lambda b: ("S", 0, b)
            for b in range(TB):
                P.dve(lambda e, b=b: e.tensor_scalar(out=S[s][:, b, :], in0=iota_f[:], scalar1=POS[:, b, ex:ex + 1],
                                                     scalar2=MSK[:, b, ex:ex + 1], op0=ALU.is_equal, op1=ALU.mult),
                      reads=["pos", "msk", "iota_f"], writes=[Sk(b)])
            bw = nbank()
            for jb in range(2):
                for b in range(TB):
                    P.pe(lambda e, jb=jb, b=b: e.matmul(fb[bw][:, 2 * jb:2 * jb + 2], S[s][:, b, jb * 128:(jb + 1) * 128],
                                                        WHL[:, b, ex, :], start=(jb == 0 and b == 0), stop=(b == TB - 1),
                                                        skip_group_check=True),
                         reads=[Sk(b), "whl0", "whl1"], writes=[fk[bw]])
            P.dve(lambda e: e.tensor_reduce(out=wsl[s][:], in_=fb[bw][:, 0:4].rearrange("p (j k) -> p j k", k=2),
                                            axis=AX.X, op=ALU.add), reads=[fk[bw]], writes=[("wsl", s)])
            for jb in range(2):
                for b in range(TB):
                    t = tb[0] if b < 8 else tb[1]
                    tkk = tk[0] if b < 8 else tk[1]
                    off = (b % 8) * 128
                    P.pe(lambda e, jb=jb, b=b, t=t, off=off: e.transpose(t[:, off:off + 128], S[s][:, b, jb * 128:(jb + 1) * 128],
                                                                        C["ident_b"][:]),
                         reads=[Sk(b), K["ident_b"]], writes=[tkk])
                P.dve(lambda e, jb=jb: e.tensor_copy(out=ST[s][:, jb, 0:1024], in_=tb[0][:, :]), reads=[tk[0]], writes=[("ST", s, jb, 0)])
                P.act(lambda e, jb=jb: e.activation(out=ST[s][:, jb, 1024:NT], in_=tb[1][:, 0:128], func=AF.Copy),
                      reads=[tk[1]], writes=[("ST", s, jb, 1)])
            for c in range(16):
                bgx = nbank()
                for b in range(TB):
                    P.pe(lambda e, c=c, b=b, bgx=bgx: e.matmul(fb[bgx][:, 0:CAP], Hb[:, b, c * 128:(c + 1) * 128], S[s][:, b, :],
                                                               start=(b == 0), stop=(b == TB - 1)),
                         reads=[("Hb", b), Sk(b)], writes=[fk[bgx]])
                if c % 2 == 0:
                    P.dve(lambda e, c=c, bgx=bgx: e.tensor_copy(out=XeT[:, c, :], in_=fb[bgx][:, 0:CAP]),
                          reads=[fk[bgx]], writes=[("XeT", s, c)])
                else:
                    P.act(lambda e, c=c, bgx=bgx: e.activation(out=XeT[:, c, :], in_=fb[bgx][:, 0:CAP], func=AF.Copy),
                          reads=[fk[bgx]], writes=[("XeT", s, c)])
        def part2(ex):
            s = ex % 2
            XeT = XeT2[s]
            gv = io["wgate"][ex].rearrange("(c p) f -> p c f", p=128)
            uv = io["wup"][ex].rearrange("(c p) f -> p c f", p=128)
            for pc in range(4):
                w = pcnt[0] % 2
                pcnt[0] += 1
                for hf in range(2):
                    P.dma(lambda e, w=w, pc=pc, hf=hf: e.dma_start(out=wgp[w][:, 8 * hf:8 * hf + 8, :],
                                                                  in_=gv[:, 8 * hf:8 * hf + 8, pc * 256:(pc + 1) * 256]),
                          writes=[("wgp", w, hf)], q="pool")
                    P.dma(lambda e, w=w, pc=pc, hf=hf: e.dma_start(out=wup[w][:, 8 * hf:8 * hf + 8, :],
                                                                  in_=uv[:, 8 * hf:8 * hf + 8, pc * 256:(pc + 1) * 256]),
                          writes=[("wup", w, hf)], q="pool")
                for fl in range(2):
                    fc = pc * 2 + fl
                    bg, bu = nbank(), nbank()
                    for c in range(16):
                        P.pe(lambda e, c=c, w=w, fl=fl, bg=bg: e.matmul(fb[bg][:, 0:CAP], wgp[w][:, c, fl * 128:(fl + 1) * 128],
                                                                        XeT[:, c, :], start=(c == 0), stop=(c == 15)),
                             reads=[("wgp", w, c // 8), ("XeT", s, c)], writes=[fk[bg]])
                    for c in range(16):
                        P.pe(lambda e, c=c, w=w, fl=fl, bu=bu: e.matmul(fb[bu][:, 0:CAP], wup[w][:, c, fl * 128:(fl + 1) * 128],
                                                                        XeT[:, c, :], start=(c == 0), stop=(c == 15)),
                             reads=[("wup", w, c // 8), ("XeT", s, c)], writes=[fk[bu]])
                    u = fc % 2
                    P.act(lambda e, u=u, bg=bg: e.activation(out=sgl[u][:], in_=fb[bg][:, 0:CAP], func=AF.Silu),
                          reads=[fk[bg]], writes=[("sgl", u)])
                    P.dve(lambda e, u=u, bu=bu, fc=fc: e.tensor_mul(out=AT[:, fc, :], in0=sgl[u][:], in1=fb[bu][:, 0:CAP]),
                          reads=[("sgl", u), fk[bu]], writes=[("AT", fc)])
        def part3(ex):
            s = ex % 2
            dv = io["wdown"][ex].rearrange("(c p) n -> p c n", p=128)
            ATk = [("AT", fc) for fc in range(8)]
            for cg in range(4):
                w = dcnt[0] % 2
                dcnt[0] += 1
                for hf in range(2):
                    P.dma(lambda e, w=w, cg=cg, hf=hf: e.dma_start(out=wdp[w][:, 4 * hf:4 * hf + 4, :],
                                                                  in_=dv[:, 4 * hf:4 * hf + 4, cg * 512:(cg + 1) * 512]),
                          writes=[("wdp", w, hf)], q="pool")
                for jb in range(2):
                    by = nbank()
                    for fc in range(8):
                        P.pe(lambda e, fc=fc, jb=jb, w=w, by=by: e.matmul(fb[by][:, :], AT[:, fc, jb * 128:(jb + 1) * 128],
                                                                          wdp[w][:, fc, :], start=(fc == 0), stop=(fc == 7)),
                             reads=[("AT", fc), ("wdp", w, fc // 4)], writes=[fk[by]])
                    if jb == 0:
                        P.act(lambda e, jb=jb, cg=cg, by=by: e.activation(out=Yb[:, jb, cg * 512:(cg + 1) * 512], in_=fb[by][:, :],
                                                                          func=AF.Copy, scale=wsl[s][:, jb:jb + 1]),
                              reads=[fk[by], ("wsl", s)], writes=[("Yb", jb, cg)])
                    else:
                        P.dve(lambda e, jb=jb, cg=cg, by=by: e.tensor_scalar(out=Yb[:, jb, cg * 512:(cg + 1) * 512], in0=fb[by][:, :],
                                                                             scalar1=wsl[s][:, jb:jb + 1], scalar2=None, op0=ALU.mult),
                              reads=[fk[by], ("wsl", s)], writes=[("Yb", jb, cg)])
        def part4(ex):
            s = ex % 2
            for cg in range(4):
                for b in range(TB):
                    bc_ = nbank()
                    for jb in range(2):
                        P.pe(lambda e, jb=jb, b=b, cg=cg, bc_=bc_: e.matmul(fb[bc_][:, :], ST[s][:, jb, b * 128:(b + 1) * 128],
                                                                            Yb[:, jb, cg * 512:(cg + 1) * 512],
                                                                            start=(jb == 0), stop=(jb == 1)),
                             reads=[("ST", s, jb, 0 if b < 8 else 1), ("Yb", jb, cg)], writes=[fk[bc_]])
                    P.dve(lambda e, b=b, cg=cg, bc_=bc_: e.tensor_add(out=X[:, b, cg * 512:(cg + 1) * 512],
                                                                      in0=X[:, b, cg * 512:(cg + 1) * 512], in1=fb[bc_][:, :]),
                          reads=[XK(b), fk[bc_]], writes=[XK(b)])

        import os
        _nex = int(os.environ.get("T_NEXP", NE))
        part1(0)
        for ex in range(_nex):
            part2(ex)
            if ex + 1 < _nex:
                part1(ex + 1)
            part3(ex)
            part4(ex)
        P.barrier()
        stD.close()

        layer_norm(io["lnf"], cx)
        write_outputs(Hb, cx)
        P.emit()


_PROGS = {}


def _prog(name):
    if name not in _PROGS:
        _PROGS[name] = {"T0": lambda: build_T(True), "A": build_A, "T": lambda: build_T(False)}[name]()
    return _PROGS[name]


def _run(name, in_maps):
    res = run_bass_kernel_spmd(_prog(name), in_maps, core_ids=list(range(NCORE)))
    return res.results


def _gather_hT(outs):
    full = np.empty((D, L), dtype=ml_dtypes.bfloat16)
    full[:, 0:128] = np.asarray(outs[0]["hT"])[:, 0:128]
    for c in range(NCORE):
        full[:, 128 + 1024 * c:128 + 1024 * (c + 1)] = np.asarray(outs[c]["hT"])[:, 128:NT]
    return full


def kernel(x, meta_tokens, ln_in_g, ln_in_b, w_in, b_forget, w_branch_sb, w_branch_fox, w_out,
           ln_mix_g, ln_mix_b, w_router, router_bias, w_gate, w_up, w_down, ln_ffn_g, ln_ffn_b):
    f32 = np.float32
    x = np.asarray(x, f32)
    w_in = np.asarray(w_in, f32)
    depth = w_in.shape[0]
    blk0 = np.concatenate([np.zeros((NPAD, D), f32), np.asarray(meta_tokens, f32)], 0)
    lng = np.stack([np.asarray(ln_in_g, f32), np.asarray(ln_in_b, f32)])
    t_in = [{"xin": np.ascontiguousarray(np.concatenate([blk0, x[0, 1024 * c:1024 * (c + 1)]], 0)), "lng": lng}
            for c in range(NCORE)]
    outs = _run("T0", t_in)
    cols = [np.concatenate([np.arange(0, 128), 128 + 1024 * c + np.arange(1024)]) for c in range(NCORE)]
    for i in range(depth):
        hT_all = _gather_hT(outs)
        a_in = []
        for c in range(NCORE):
            s = slice(c * 128, (c + 1) * 128)
            wA = np.concatenate([w_in[i][:, 0:1024][:, s], w_in[i][:, 1024:2048][:, s],
                                 w_in[i][:, 3072:4096][:, s], w_in[i][:, 4096:5120][:, s],
                                 w_in[i][:, 2048:3072][:, s], w_in[i][:, 5120:6144][:, s],
                                 w_in[i][:, 6144 + c:6145 + c]], 1)
            a_in.append({"hT": hT_all, "wA": np.ascontiguousarray(wA),
                         "bf": np.asarray(b_forget, f32)[i, c].reshape(1, 1)})
        a_out = _run("A", a_in)
        del a_in, hT_all
        Y = np.empty((D, L), dtype=ml_dtypes.bfloat16)
        for c in range(NCORE):
            y = np.asarray(a_out[c]["yT"])
            Y[c * 128:(c + 1) * 128] = y[0:128]
            Y[1024 + c * 128:1024 + (c + 1) * 128] = y[128:256]
        wg = np.ascontiguousarray(w_in[i][:, 6152:6152 + 2 * D])
        shared = {"wg": wg, "wbs": np.asarray(w_branch_sb, f32)[i], "wbf": np.asarray(w_branch_fox, f32)[i],
                  "wo": np.asarray(w_out, f32)[i],
                  "lnm": np.stack([np.asarray(ln_mix_g, f32)[i], np.asarray(ln_mix_b, f32)[i]]),
                  "wr": np.asarray(w_router, f32), "rb": np.asarray(router_bias, f32).reshape(1, NE),
                  "wgate": np.asarray(w_gate, f32)[i], "wup": np.asarray(w_up, f32)[i],
                  "wdown": np.asarray(w_down, f32)[i],
                  "lnf": np.stack([np.asarray(ln_ffn_g, f32)[i], np.asarray(ln_ffn_b, f32)[i]])}
        t_in = []
        for c in range(NCORE):
            m = dict(shared)
            m["hprev"] = np.asarray(outs[c]["h"])
            m["hTp"] = np.asarray(outs[c]["hT"])
            m["yT"] = np.ascontiguousarray(Y[:, cols[c]])
            t_in.append(m)
        outs = _run("T", t_in)
        del t_in, Y
    out = np.empty((1, SEQ, D), f32)
    for c in range(NCORE):
        out[0, 1024 * c:1024 * (c + 1)] = np.asarray(outs[c]["h"])[128:NT]
    return out
```
